# Optimizing a Trainium2 kernel written in Bass

```python
import jax, jax.numpy as jnp
from jax import lax
import numpy as np

D_MODEL = 1024
BATCH = 16
SEQ = 2048
DEPTH = 2

MIX_WIDTH = D_MODEL
GM_HEADS = 4
GM_WIDTH = MIX_WIDTH // 2
GM_HEAD_DIM = GM_WIDTH // GM_HEADS
GM_CHUNK = 128
GLA_HEADS = 4
GLA_WIDTH = MIX_WIDTH - GM_WIDTH
GLA_DV = GLA_WIDTH // GLA_HEADS
GLA_KEY_WIDTH = GLA_WIDTH // 2
GLA_DK = GLA_KEY_WIDTH // GLA_HEADS
GLA_GATE_RANK = 16
GLA_TAU = 16.0
GLA_CHUNK = 64
IN_COLS = 2 * GM_WIDTH + 2 * GLA_KEY_WIDTH + 2 * GLA_WIDTH + GLA_GATE_RANK
D_FF = 2816
N_EXPERTS = 8
TOP_K = 2
D_FF_EXPERT = 3584
N_DENSE = (DEPTH + 1) // 2
N_MOE = DEPTH // 2
LN_EPS = 1e-5
RMS_EPS = 1e-6
DEEPNORM_ALPHA = (2 * DEPTH) ** 0.25
DEEPNORM_BETA = (8 * DEPTH) ** -0.25

kernel_name = 'hybrid_gmlp_gla_deepnorm_moe'


def layer_norm(x, g, b):
    xf = x.astype(jnp.float32)
    mu = jnp.mean(xf, axis=-1, keepdims=True)
    var = jnp.mean(jnp.square(xf - mu), axis=-1, keepdims=True)
    y = (xf - mu) * lax.rsqrt(var + LN_EPS) * g.astype(jnp.float32) + b.astype(jnp.float32)
    return y.astype(x.dtype)


def chunked_spatial_gating(u, v, w_s, b_s, ln_g, ln_b):
    B, S, _ = u.shape
    n = S // GM_CHUNK
    v = layer_norm(v.reshape(B, S, GM_HEADS, GM_HEAD_DIM),
                   ln_g.reshape(GM_HEADS, GM_HEAD_DIM), ln_b.reshape(GM_HEADS, GM_HEAD_DIM))
    v = v.reshape(B, n, GM_CHUNK, GM_HEADS, GM_HEAD_DIM)
    causal = jnp.tril(jnp.ones((GM_CHUNK, GM_CHUNK), dtype=bool))
    w = jnp.where(causal[None], w_s, 0)
    s = jnp.einsum('hts,bnshc->bnthc', w, v) + b_s.T[None, None, :, :, None]
    return u * s.reshape(B, S, GM_WIDTH)


def gla_chunked(q, k, v, log_a):
    B, S, H, dk = q.shape
    dv = v.shape[-1]
    n = S // GLA_CHUNK

    def to_chunks(t):
        return t.astype(jnp.float32).reshape(B, n, GLA_CHUNK, H, t.shape[-1]).transpose(1, 0, 3, 2, 4)

    q, k, v, log_a = to_chunks(q), to_chunks(k), to_chunks(v), to_chunks(log_a)
    q = q * (dk ** -0.5)
    b = jnp.cumsum(log_a, axis=3)
    b_last = b[:, :, :, -1:, :]
    q_dec = q * jnp.exp(b)
    k_inv = k * jnp.exp(-b)
    k_dec = k * jnp.exp(b_last - b)
    causal = jnp.tril(jnp.ones((GLA_CHUNK, GLA_CHUNK), dtype=bool))
    scores = jnp.where(causal, jnp.einsum('nbhid,nbhjd->nbhij', q_dec, k_inv), 0.0)
    o_intra = jnp.einsum('nbhij,nbhje->nbhie', scores, v)

    def step(state, inp):
        q_c, k_c, v_c, decay_c = inp
        o = jnp.einsum('bhid,bhde->bhie', q_c, state)
        state = state * decay_c[..., 0, :, None] + jnp.einsum('bhjd,bhje->bhde', k_c, v_c)
        return state, o

    state0 = jnp.zeros((B, H, dk, dv), jnp.float32)
    _, o_inter = lax.scan(step, state0, (q_dec, k_dec, v, jnp.exp(b_last)))
    o = o_intra + o_inter
    return o.transpose(1, 0, 3, 2, 4).reshape(B, S, H, dv)


def hybrid_mixer(x, w_in, gm_ws, gm_bs, gm_ln_g, gm_ln_b, gla_wa2, gla_ba, gla_norm_g, w_out):
    B, S, _ = x.shape
    proj = x @ w_in
    o1 = GM_WIDTH
    o2 = o1 + GM_WIDTH
    o3 = o2 + GLA_KEY_WIDTH
    o4 = o3 + GLA_KEY_WIDTH
    o5 = o4 + GLA_WIDTH
    o6 = o5 + GLA_WIDTH
    gm_u, gm_v, q, k, v, g, a_lr = jnp.split(proj, [o1, o2, o3, o4, o5, o6], axis=-1)
    gm_u = jax.nn.gelu(gm_u, approximate=False)
    gm_v = jax.nn.gelu(gm_v, approximate=False)
    out_a = chunked_spatial_gating(gm_u, gm_v, gm_ws, gm_bs, gm_ln_g, gm_ln_b)
    log_a = jax.nn.log_sigmoid((a_lr @ gla_wa2 + gla_ba).astype(jnp.float32)) / GLA_TAU
    o = gla_chunked(q.reshape(B, S, GLA_HEADS, GLA_DK), k.reshape(B, S, GLA_HEADS, GLA_DK),
                    v.reshape(B, S, GLA_HEADS, GLA_DV), log_a.reshape(B, S, GLA_HEADS, GLA_DK))
    o = o * lax.rsqrt(jnp.mean(jnp.square(o), axis=-1, keepdims=True) + RMS_EPS)
    o = o * gla_norm_g.reshape(GLA_HEADS, GLA_DV).astype(jnp.float32)
    out_b = o.reshape(B, S, GLA_WIDTH).astype(x.dtype) * jax.nn.silu(g)
    return jnp.concatenate([out_a, out_b], axis=-1) @ w_out


def swiglu(x, w_gate, w_up, w_down):
    return (jax.nn.silu(x @ w_gate) * (x @ w_up)) @ w_down


def moe_swiglu(x, w_router, e_gate, e_up, e_down):
    B, S, D = x.shape
    t = x.reshape(B * S, D)
    logits = (t @ w_router).astype(jnp.float32)
    top_val, top_idx = lax.top_k(logits, TOP_K)
    top_w = jax.nn.softmax(top_val, axis=-1)
    gates = jnp.sum(jax.nn.one_hot(top_idx, N_EXPERTS, dtype=jnp.float32) * top_w[..., None], axis=1)
    gates = gates.astype(t.dtype)
    out = jnp.zeros_like(t)
    for e in range(N_EXPERTS):
        out = out + gates[:, e:e + 1] * swiglu(t, e_gate[e], e_up[e], e_down[e])
    return out.reshape(B, S, D)


def setup_inputs(seed: int = 0) -> dict:
    key = jax.random.key(seed)
    ks = jax.random.split(key, 22)
    nrm = jax.random.normal
    f32 = jnp.float32
    D = D_MODEL
    return {
        'x': nrm(ks[0], (BATCH, SEQ, D), f32),
        'w_in': nrm(ks[1], (DEPTH, D, IN_COLS), f32) * D ** -0.5,
        'gm_ws': nrm(ks[2], (DEPTH, GM_HEADS, GM_CHUNK, GM_CHUNK), f32) * GM_CHUNK ** -0.5,
        'gm_bs': 1.0 + 0.1 * nrm(ks[3], (DEPTH, GM_HEADS, GM_CHUNK), f32),
        'gm_ln_g': 1.0 + 0.1 * nrm(ks[4], (DEPTH, GM_WIDTH), f32),
        'gm_ln_b': 0.02 * nrm(ks[5], (DEPTH, GM_WIDTH), f32),
        'gla_wa2': nrm(ks[6], (DEPTH, GLA_GATE_RANK, GLA_KEY_WIDTH), f32) * GLA_GATE_RANK ** -0.5,
        'gla_ba': 0.1 * nrm(ks[7], (DEPTH, GLA_KEY_WIDTH), f32),
        'gla_norm_g': 1.0 + 0.1 * nrm(ks[8], (DEPTH, GLA_WIDTH), f32),
        'w_out': nrm(ks[9], (DEPTH, MIX_WIDTH, D), f32) * MIX_WIDTH ** -0.5 * DEEPNORM_BETA,
        'ln_mix_g': 1.0 + 0.1 * nrm(ks[10], (DEPTH, D), f32),
        'ln_mix_b': 0.02 * nrm(ks[11], (DEPTH, D), f32),
        'ffn_w_gate': nrm(ks[12], (N_DENSE, D, D_FF), f32) * D ** -0.5,
        'ffn_w_up': nrm(ks[13], (N_DENSE, D, D_FF), f32) * D ** -0.5,
        'ffn_w_down': nrm(ks[14], (N_DENSE, D_FF, D), f32) * D_FF ** -0.5 * DEEPNORM_BETA,
        'router_w': nrm(ks[15], (N_MOE, D, N_EXPERTS), f32) * D ** -0.5,
        'exp_w_gate': nrm(ks[16], (N_MOE, N_EXPERTS, D, D_FF_EXPERT), f32) * D ** -0.5,
        'exp_w_up': nrm(ks[17], (N_MOE, N_EXPERTS, D, D_FF_EXPERT), f32) * D ** -0.5,
        'exp_w_down': nrm(ks[18], (N_MOE, N_EXPERTS, D_FF_EXPERT, D), f32) * D_FF_EXPERT ** -0.5 * DEEPNORM_BETA,
        'ln_ffn_g': 1.0 + 0.1 * nrm(ks[19], (DEPTH, D), f32),
        'ln_ffn_b': 0.02 * nrm(ks[20], (DEPTH, D), f32),
    }


def reference(x, w_in, gm_ws, gm_bs, gm_ln_g, gm_ln_b, gla_wa2, gla_ba, gla_norm_g, w_out,
              ln_mix_g, ln_mix_b, ffn_w_gate, ffn_w_up, ffn_w_down, router_w,
              exp_w_gate, exp_w_up, exp_w_down, ln_ffn_g, ln_ffn_b):
    for layer in range(DEPTH):
        h = hybrid_mixer(x, w_in[layer], gm_ws[layer], gm_bs[layer], gm_ln_g[layer], gm_ln_b[layer],
                         gla_wa2[layer], gla_ba[layer], gla_norm_g[layer], w_out[layer])
        x = layer_norm(DEEPNORM_ALPHA * x + h, ln_mix_g[layer], ln_mix_b[layer])
        i = layer // 2
        if layer % 2 == 0:
            f = swiglu(x, ffn_w_gate[i], ffn_w_up[i], ffn_w_down[i])
        else:
            f = moe_swiglu(x, router_w[i], exp_w_gate[i], exp_w_up[i], exp_w_down[i])
        x = layer_norm(DEEPNORM_ALPHA * x + f, ln_ffn_g[layer], ln_ffn_b[layer])
    return x
```

```python
import numpy as np
import concourse.bass as bass
import concourse.mybir as mybir
from concourse.bass_utils import run_bass_kernel_spmd

F32 = mybir.dt.float32
BF16 = mybir.dt.bfloat16
AF = mybir.ActivationFunctionType
ALU = mybir.AluOpType
AX = mybir.AxisListType

D = 1024
KC = 8
DEPTH = 2
IN_COLS = 2576
D_FF = 2816
D_FFE = 3584
NE = 8
ALPHA = float((2 * DEPTH) ** 0.25)
LN_EPS = 1e-5
RMS_EPS = 1e-6
TT = 512
NCORES = 8

ENGS = ["pe", "act", "dve", "pool", "sp"]


class Slot:
    def __init__(self, prog, name):
        self.sem = prog.nc.alloc_semaphore(name)
        self.n = 0

    def tok(self):
        return (self.sem, self.n * 16)


class Prog:
    def __init__(self, nc):
        self.nc = nc
        self.ops = {e: [] for e in ENGS}
        self.sems = {e: nc.alloc_semaphore("sem_" + e) for e in ENGS}
        self.cnt = {e: 0 for e in ENGS}
        self.known = {e: {} for e in ENGS}
        self.nslots = 0

    def slot(self, name=None):
        self.nslots += 1
        return Slot(self, name or f"dslot{self.nslots}")

    def _flat(self, waits, acc):
        for t in waits:
            if t is None:
                continue
            if isinstance(t, (list, tuple)) and (len(t) == 0 or isinstance(t[0], (list, tuple)) or t[0] is None):
                self._flat(t, acc)
            else:
                acc[t[0]] = max(acc.get(t[0], 0), t[1])

    def _waits(self, eng, waits):
        w = {}
        self._flat(waits, w)
        out = []
        kn = self.known[eng]
        for s, v in w.items():
            if v <= 0 or kn.get(s, 0) >= v:
                continue
            kn[s] = v
            out.append((s, v))
        return out

    def op(self, eng, fn, waits=(), sig=True):
        w = self._waits(eng, waits)
        tok = None
        if sig:
            self.cnt[eng] += 1
            tok = (self.sems[eng], self.cnt[eng])
        self.ops[eng].append((w, fn, 1 if sig else 0, self.sems[eng]))
        return tok

    def dma(self, q, out, in_, slot, waits=(), **kw):
        w = self._waits(q, waits)
        slot.n += 1
        def fn(e):
            o = out(e) if callable(out) else out
            i = in_(e) if callable(in_) else in_
            try:
                return e.dma_start(out=o, in_=i, **kw)
            except Exception:
                print("DMA FAIL", q, o, i)
                raise
        self.ops[q].append((w, fn, 16, slot.sem))
        return slot.tok()

    def raw(self, q, fn, slot, waits=()):
        w = self._waits(q, waits)
        slot.n += 1
        self.ops[q].append((w, fn, 16, slot.sem))
        return slot.tok()

    def emit(self, final_waits):
        nc = self.nc
        engmap = {"pe": "tensor", "act": "scalar", "dve": "vector", "pool": "gpsimd", "sp": "sync"}
        with nc.Block() as block:
            for e in ENGS:
                ops = self.ops[e]
                extra = self._waits(e, final_waits) if e == "sp" else []

                def body(engine, ops=ops, extra=extra):
                    for (w, fn, inc, sem) in ops:
                        for (s, v) in w:
                            engine.wait_ge(s, v)
                        ins = fn(engine)
                        if inc:
                            ins.then_inc(sem, inc)
                    for (s, v) in extra:
                        engine.wait_ge(s, v)

                getattr(block, engmap[e])(body)


class Ring:
    def __init__(self, tiles):
        self.tiles = tiles
        self.n = len(tiles)
        self.free = [[] for _ in tiles]
        self.i = 0

    def get(self):
        k = self.i % self.n
        self.i += 1
        return k, self.tiles[k], self.free[k]

    def rel(self, k, toks):
        self.free[k] = [t for t in toks if t is not None]


def make_consts(NT=4096):
    j = np.arange(128)[:, None]
    i = np.arange(128)[None, :]
    same = (j // 64) == (i // 64)
    ident = np.eye(128, dtype=np.float32)
    tri = ((j <= i) & same).astype(np.float32)
    triu = ((j > i) & same).astype(np.float32)
    causal = (j <= i).astype(np.float32)
    ones = np.ones((128, 128), np.float32)
    slt = (j < i).astype(np.float32)
    jrow = np.broadcast_to(np.arange(32, dtype=np.float32)[None, :], (128, 32))
    tokidx = (np.arange(32)[None, :] * 128 + np.arange(128)[:, None]).astype(np.float32)
    p_ = np.arange(128)[:, None]
    iota_g = (np.arange(8)[None, :] * 128 + p_).astype(np.float32)
    iota_d = (np.arange(28)[None, :] * 128 + p_).astype(np.float32)
    nslot = (2 * NT + 8 * (TT - 1)) // TT
    padq = np.zeros((128, 96), np.float32)
    padq[:, :nslot * 4] = 2 * NT + (p_ * (nslot * 4) + np.arange(nslot * 4)[None, :])
    return np.ascontiguousarray(np.concatenate([ident, tri, tri, tri, tri, triu, causal, ones, slt, jrow, tokidx, iota_g, iota_d, padq], axis=1))


C_ID, C_TRI4, C_TRIU, C_CAUS, C_ONES, C_SLT, C_JROW, C_TOK, C_IG, C_IDN, C_PADQ, C_N = 0, 128, 640, 768, 896, 1024, 1152, 1184, 1216, 1224, 1252, 1348
I32 = mybir.dt.int32


def build(NT, SEQ_T, stop=None, debug=False):
    assert NT % TT == 0 and SEQ_T % TT == 0
    ntiles = NT // TT
    nc = bass.Bass("TRN2", target_bir_lowering=False)

    def din(name, shape):
        return nc.dram_tensor(name, list(shape), F32, kind="ExternalInput").ap()

    x_d = din("x", [NT, D])
    w_in_d = din("w_in", [DEPTH, D, IN_COLS])
    gm_ws_d = din("gm_ws", [DEPTH, 4, 128, 128])
    gm_bs_d = din("gm_bs", [DEPTH, 512])
    gm_ln_g_d = din("gm_ln_g", [DEPTH, 512])
    gm_ln_b_d = din("gm_ln_b", [DEPTH, 512])
    wa2_d = din("gla_wa2", [DEPTH, 16, 256])
    ba_d = din("gla_ba", [DEPTH, 256])
    gng_d = din("gla_norm_g", [DEPTH, 512])
    w_out_d = din("w_out", [DEPTH, D, D])
    ln_mix_g_d = din("ln_mix_g", [DEPTH, D])
    ln_mix_b_d = din("ln_mix_b", [DEPTH, D])
    fg_d = din("ffn_w_gate", [1, D, D_FF])
    fu_d = din("ffn_w_up", [1, D, D_FF])
    fd_d = din("ffn_w_down", [1, D_FF, D])
    rw_d = din("router_w", [1, D, NE])
    eg_d = din("exp_w_gate", [1, NE, D, D_FFE])
    eu_d = din("exp_w_up", [1, NE, D, D_FFE])
    ed_d = din("exp_w_down", [1, NE, D_FFE, D])
    ln_ffn_g_d = din("ln_ffn_g", [DEPTH, D])
    ln_ffn_b_d = din("ln_ffn_b", [DEPTH, D])
    consts_d = din("consts", [128, C_N])
    out_d = nc.dram_tensor("out", [NT, D], F32, kind="ExternalOutput").ap()

    P = Prog(nc)
    sb = nc.alloc_sbuf_tensor

    consts = sb("consts_sb", [128, C_N], F32)
    ident = consts[:, C_ID:C_ID + 128]
    tri4 = consts[:, C_TRI4:C_TRI4 + 512]
    tri = consts[:, C_TRI4:C_TRI4 + 128]
    triu = consts[:, C_TRIU:C_TRIU + 128]
    causal = consts[:, C_CAUS:C_CAUS + 128]
    ones = consts[:, C_ONES:C_ONES + 128]
    slt = consts[:, C_SLT:C_SLT + 128]
    jrow = consts[:, C_JROW:C_JROW + 32]

    wmT = [sb(f"wmT{L}", [128, 4, 128], BF16) for L in range(DEPTH)]
    bB = [sb(f"bB{L}", [128, 512], F32) for L in range(DEPTH)]
    lnG = [sb(f"lnG{L}", [128, 512], F32) for L in range(DEPTH)]
    lnBt = [sb(f"lnBt{L}", [128, 512], F32) for L in range(DEPTH)]
    wa2 = [sb(f"wa2{L}", [17, 256], F32) for L in range(DEPTH)]
    gn = [sb(f"gn{L}", [128, 4], F32) for L in range(DEPTH)]
    rw = sb("rw_sb", [128, KC, NE], F32)
    lnp_ring = Ring([sb(f"lnp{i}", [128, 2, D], F32) for i in range(1)])
    lnp_slots = [P.slot(f"lnp_s{i}") for i in range(1)]

    WAall = sb("WAall", [128, 4 * KC * 528], BF16)
    WA = Ring([WAall[:, i * KC * 528:(i + 1) * KC * 528].rearrange("p (k c) -> p k c", c=528) for i in range(4)])
    WA_s = [P.slot(f"WA_s{i}") for i in range(4)]
    WAm = Ring([WAall[:, i * 4096:(i + 1) * 4096].rearrange("p (k c) -> p k c", c=512) for i in range(4)])
    cur_ring = {"WA": WA}
    WB = Ring([sb(f"WB{i}", [128, 4, D], BF16) for i in range(2)])
    WB_s = [P.slot(f"WB_s{i}") for i in range(2)]

    x_res = sb("x_res", [128, 4, D], F32)
    xT = [sb(f"xT{i}", [128, KC, TT], BF16) for i in range(2)]
    ovl = sb("ovl", [128, 28 * TT], BF16)
    hT = ovl[:, :].rearrange("p (c t) -> p c t", t=TT)
    uT = ovl[:, 0:2048].rearrange("p (c t) -> p c t", t=TT)
    sgT = ovl[:, 2048:4096].rearrange("p (c t) -> p c t", t=TT)
    vln = ovl[:, 4096:6144].rearrange("p (c t) -> p c t", t=512)
    v_tok = ovl[:, 6144:8192].rearrange("p (c t) -> p c t", t=512)
    catT = ovl[:, 8192:12288].rearrange("p (c t) -> p c t", t=TT)
    qk_buf = sb("qk_buf", [128, 4096], F32)
    qT = qk_buf[0:64, 0:2048].rearrange("p (c t) -> p c t", t=TT)
    kT = qk_buf[0:64, 2048:4096].rearrange("p (c t) -> p c t", t=TT)
    xg4 = qk_buf[:, :].rearrange("p (c t) -> p c t", t=D)
    mixF = sb("mixF", [128, 6400], F32)
    k_tok = mixF[:, 4864:5888].rearrange("p (c t) -> p c t", t=256)
    a_aug = mixF[0:17, 5888:6400]
    S = [sb(f"S{L}", [64, 4, 128], F32) for L in range(DEPTH)]
    S_bf = [sb(f"Sbf{L}", [64, 4, 128], BF16) for L in range(DEPTH)]
    scrA = sb("scrA", [128, 2048], F32)
    vg4 = scrA[:, :].rearrange("p (c t) -> p c t", t=512)
    st6 = sb("st6", [128, 16, 6], F32)
    mv = sb("mv", [128, 16, 2], F32)
    rstd16 = sb("rstd16", [128, 16], F32)
    lnv16 = sb("lnv16", [128, 16], F32)
    eps_ln = sb("eps_ln", [128, 1], F32)
    eps_rms = sb("eps_rms", [128, 1], F32)
    one_t = sb("one_t", [128, 1], F32)
    ln_l = sb("ln_l", [128, 1], F32)
    rr_l = mixF[:, 2048:2560]
    vhat = mixF[:, 0:512]
    tmp_sa = mixF[:, 512:1024]
    e1 = mixF[:, 3072:3328]
    Lsp = mixF[:, 3328:3584]
    Eq = mixF[0:64, 3840:4352].rearrange("p (c t) -> p c t", t=128)
    Ek = mixF[0:64, 4352:4864].rearrange("p (c t) -> p c t", t=128)
    Er = mixF[:, 3584:3840]
    qd = sb("qd", [64, 4, 128], BF16)
    kinv = sb("kinv", [64, 4, 128], BF16)
    kdec = sb("kdec", [128, 256], BF16)
    scm = sb("scm", [128, 4, 128], BF16)
    ones_bf = sb("ones_bf", [128, 128], BF16)
    sq_bf = sb("sq_bf", [128, 512], BF16)
    sq = mixF[:, 1024:1536]
    rr = mixF[:, 1536:2048]
    t1 = mixF[:, 2560:3072]
    ln_st = sb("ln_st", [128, 2, 6], F32)
    ln_mv = sb("ln_mv", [128, 2], F32)
    ln_r = sb("ln_r", [128, 1], F32)
    sg_ring = Ring([sb(f"sg{i}", [128, TT], F32) for i in range(2)])
    x2T = scrA[:, 0:1024].rearrange("p (c t) -> p c t", t=128)
    lg = sb("lg", [128, NE], F32)
    m1 = sb("m1", [128, 1], F32)
    nm1 = sb("nm1", [128, 1], F32)
    m2 = sb("m2", [128, 1], F32)
    eq1 = sb("eq1", [128, NE], F32)
    l2 = sb("l2", [128, NE], F32)
    sel = sb("sel", [128, NE], F32)
    wdesc = sb("wdesc", [128, NE], F32)
    mt = sb("mt", [128, 1], F32)
    ex = sb("ex", [128, NE], F32)
    e2 = sb("e2", [128, 1], F32)
    rden = sb("rden", [128, 1], F32)
    gates = sb("gates", [128, 4, NE], F32)

    PS = Ring([nc.alloc_psum_tensor(f"ps{i}", [128, 512], F32) for i in range(8)])

    def PE(fn, waits=(), sig=False):
        return P.op("pe", fn, waits, sig)

    def ACT(fn, waits=()):
        return P.op("act", fn, waits, True)

    def DVE(fn, waits=()):
        return P.op("dve", fn, waits, True)

    def POOL(fn, waits=()):
        return P.op("pool", fn, waits, True)

    def mm_group(out, pairs, waits):
        n = len(pairs)
        tok = None
        for i, (l, r) in enumerate(pairs):
            tok = PE(lambda e, l=l, r=r, i=i: e.matmul(out, l, r, start=(i == 0), stop=(i == n - 1)),
                     waits if i == 0 else (), sig=(i == n - 1))
        return tok

    s_const = P.slot("s_const")
    P.dma("sp", consts[:], consts_d, s_const)
    gmw_raw = t1[:, :].rearrange("p (c t) -> p c t", t=128)
    gn_raw = sb("gn_raw", [128, 4], F32)
    setup_toks = []
    for L in range(DEPTH):
        P.dma("sp", bB[L][:], gm_bs_d[L].partition_broadcast(128), s_const)
        P.dma("sp", lnG[L][:], gm_ln_g_d[L].partition_broadcast(128), s_const)
        P.dma("sp", lnBt[L][:], gm_ln_b_d[L].partition_broadcast(128), s_const)
        P.dma("sp", wa2[L][0:16, :], wa2_d[L], s_const)
        P.dma("sp", wa2[L][16:17, :], ba_d[L:L + 1, :], s_const)
    P.dma("sp", rw[:], rw_d[0].rearrange("(kc p) e -> p kc e", p=128), s_const)
    c_tok = s_const.tok()
    caus4 = sq
    ones_bf_tok = POOL(lambda e: e.tensor_copy(out=ones_bf[:], in_=ones), [c_tok])
    cz = [POOL(lambda e, h=h: e.tensor_copy(out=caus4[:, h * 128:(h + 1) * 128], in_=causal), [c_tok]) for h in range(4)]
    prev = [c_tok]
    for L in range(DEPTH):
        s_l = P.slot(f"s_setup{L}")
        P.dma("sp", gmw_raw[:], gm_ws_d[L].rearrange("h t s -> t h s"), s_l, waits=prev)
        P.dma("sp", gn_raw[:], gng_d[L].rearrange("(h e) -> e h", e=128), s_l, waits=prev, allow_slow_non_contiguous=True)
        lt = s_l.tok()
        k, bank, fr = PS.get()
        tk = None
        for h in range(4):
            tk = PE(lambda e, h=h, bank=bank: e.transpose(bank[:, h * 128:(h + 1) * 128], gmw_raw[:, h, :], ident),
                    [lt, c_tok, fr], sig=(h == 3))
        t_a = DVE(lambda e, L=L, bank=bank: e.tensor_tensor(out=wmT[L][:].rearrange("p h t -> p (h t)"), in0=bank[:], in1=caus4[:], op=ALU.mult),
                  [tk, cz])
        PS.rel(k, [t_a])
        t_b = DVE(lambda e, L=L: e.tensor_scalar(out=gn[L][:], in0=gn_raw[:], scalar1=float(np.sqrt(128.0)), scalar2=None,
                                                  op0=ALU.mult), [lt])
        prev = [t_a, t_b]
        setup_toks += [t_a, t_b]
    setup_toks.append(c_tok)
    setup_toks += cz

    w_in_s = nc.dram_tensor("w_in_scr", [DEPTH, D, IN_COLS], BF16).ap()
    w_out_s = nc.dram_tensor("w_out_scr", [DEPTH, D, D], BF16).ap()
    fg_s = nc.dram_tensor("fg_scr", [D, D_FF], BF16).ap()
    fu_s = nc.dram_tensor("fu_scr", [D, D_FF], BF16).ap()
    fd_s = nc.dram_tensor("fd_scr", [D_FF, D], BF16).ap()
    s_cast = P.slot("s_cast")
    cast_jobs = []
    for L_ in range(DEPTH):
        cast_jobs.append((w_in_s[L_].rearrange("(a p) c -> p a c", p=128), w_in_d[L_].rearrange("(a p) c -> p a c", p=128)))
        cast_jobs.append((w_out_s[L_].rearrange("(a p) c -> p a c", p=128), w_out_d[L_].rearrange("(a p) c -> p a c", p=128)))
    cast_jobs.append((fg_s.rearrange("(a p) c -> p a c", p=128), fg_d[0].rearrange("(a p) c -> p a c", p=128)))
    cast_jobs.append((fu_s.rearrange("(a p) c -> p a c", p=128), fu_d[0].rearrange("(a p) c -> p a c", p=128)))
    cast_jobs.append((fd_s.rearrange("(a p) c -> p a c", p=128), fd_d[0].rearrange("(a p) c -> p a c", p=128)))

    hook = {"n": 0, "deferred": [], "it": 0}

    def issue_pre(n):
        if stop is not None or hook.get("pre") is None:
            return
        jobs, st_ = hook["pre"]
        while n > 0 and st_["left"] > 0 and jobs:
            o_, i_ = jobs.pop(0)
            P.dma("pool", o_, i_, hook["pre_slot"])
            st_["left"] -= 1
            n -= 1

    def wa_hook():
        hook["n"] += 1
        if hook["n"] % 2 == 0:
            issue_pre(1)
        if hook["n"] == 4 and hook["deferred"]:
            for f in hook["deferred"]:
                f()
            hook["deferred"] = []

    def load_WA(src_ap, ncols, _prefetch=False):
        if not _prefetch and hook.get("wa_pre"):
            r_ = hook["wa_pre"].pop(0)
            wa_hook()
            return r_
        ringA = cur_ring["WA"]
        k, tile, fr = ringA.get()
        if hook["it"] > 0:
            fr = list(fr) + [s_cast.tok()]
        if isinstance(src_ap, tuple) and src_ap[0] == "ind2":
            _, dram2d, idx_ap = src_ap
            P.raw("pool", lambda e: e.indirect_dma_start(out=tile[:].rearrange("p k c -> p (k c)"), out_offset=None, in_=dram2d,
                                                         in_offset=bass.IndirectOffsetOnAxis(ap=idx_ap, axis=0)), WA_s[k], waits=fr)
            if not _prefetch:
                wa_hook()
            return k, tile, WA_s[k].tok()
        if isinstance(src_ap, tuple):
            _, dram2d, idx_ap, col0 = src_ap
            for kc in range(KC):
                P.raw("pool", lambda e, kc=kc: e.indirect_dma_start(out=tile[:, kc, 0:ncols], out_offset=None, in_=dram2d,
                                                                   in_offset=bass.IndirectOffsetOnAxis(ap=idx_ap[:, kc:kc + 1], axis=0), element_offset=col0),
                      WA_s[k], waits=fr)
            wa_hook()
            return k, tile, WA_s[k].tok()
        if callable(src_ap):
            src = lambda e: src_ap(e).rearrange("(kc p) c -> p kc c", p=128)
        else:
            src = src_ap.rearrange("(kc p) c -> p kc c", p=128)
        P.dma("pool", tile[:, :, 0:ncols], src, WA_s[k], waits=fr)
        wa_hook()
        return k, tile, WA_s[k].tok()

    def load_WB(src_ap, nchunks):
        k, tile, fr = WB.get()
        if hook["it"] > 0:
            fr = list(fr) + [s_cast.tok()]
        if isinstance(src_ap, tuple) and src_ap[0] == "ind2":
            _, dram2d, idx_ap = src_ap
            P.raw("pool", lambda e: e.indirect_dma_start(out=tile[:].rearrange("p k c -> p (k c)"), out_offset=None, in_=dram2d,
                                                         in_offset=bass.IndirectOffsetOnAxis(ap=idx_ap, axis=0)), WB_s[k], waits=fr)
            return k, tile, WB_s[k].tok()
        if isinstance(src_ap, tuple):
            _, dram2d, idx_ap, _c = src_ap
            for jj in range(nchunks):
                P.raw("pool", lambda e, jj=jj: e.indirect_dma_start(out=tile[:, jj, :], out_offset=None, in_=dram2d,
                                                                   in_offset=bass.IndirectOffsetOnAxis(ap=idx_ap[:, jj:jj + 1], axis=0)),
                      WB_s[k], waits=fr)
            return k, tile, WB_s[k].tok()
        if callable(src_ap):
            src = lambda e: src_ap(e).rearrange("(c p) d -> p c d", p=128)
        else:
            src = src_ap.rearrange("(c p) d -> p c d", p=128)
        P.dma("pool", tile[:, 0:nchunks, :], src, WB_s[k], waits=fr)
        return k, tile, WB_s[k].tok()

    def load_lnp(g_ap, b_ap):
        k, tile, fr = lnp_ring.get()
        P.dma("sp", tile[:, 0, :], g_ap.partition_broadcast(128), lnp_slots[k], waits=fr)
        P.dma("sp", tile[:, 1, :], b_ap.partition_broadcast(128), lnp_slots[k], waits=fr)
        return k, tile, lnp_slots[k].tok()

    st = {"xres": [[] for _ in range(4)],
          "xT_tok": [[], []]}

    ln_st4 = sb("ln_st4", [128, 4, 2, 6], F32)
    ln_mv4 = sb("ln_mv4", [128, 4, 2], F32)
    ln_r4 = sb("ln_r4", [128, 4], F32)
    ln_l4 = sb("ln_l4", [128, 4], F32)
    ln_rd = [None] * 4

    def ln_A1(s, y_tok):
        ta = DVE(lambda e: e.bn_stats(out=ln_st4[:, s, 0, :], in_=x_res[:, s, 0:512]), [y_tok, ln_rd[s]])
        tb = DVE(lambda e: e.bn_stats(out=ln_st4[:, s, 1, :], in_=x_res[:, s, 512:1024]), [y_tok])
        tc_ = DVE(lambda e: e.bn_aggr(out=ln_mv4[:, s, :], in_=ln_st4[:, s, :, :].rearrange("p a b -> p (a b)")), [ta, tb])
        td0 = ACT(lambda e: e.activation(out=ln_l4[:, s:s + 1], in_=ln_mv4[:, s, 1:2], func=AF.Ln, bias=eps_ln[:, 0:1]), [tc_, eps_tok])
        td = ACT(lambda e: e.activation(out=ln_r4[:, s:s + 1], in_=ln_l4[:, s:s + 1], func=AF.Exp, scale=-0.5), [td0])
        return td

    def ln_partA(s, y_tok, lnp, lnp_tok):
        return ln_A2(s, ln_A1(s, y_tok), lnp, lnp_tok)

    def ln_A2(s, td, lnp, lnp_tok):
        xs = x_res[:, s, :]
        te = DVE(lambda e: e.tensor_scalar(out=xs, in0=xs, scalar1=ln_mv4[:, s, 0:1], scalar2=ln_r4[:, s:s + 1], op0=ALU.subtract, op1=ALU.mult), [td])
        ln_rd[s] = te
        h0, h1 = slice(0, 512), slice(512, 1024)
        tf0 = DVE(lambda e: e.tensor_tensor(out=x_res[:, s, h0], in0=x_res[:, s, h0], in1=lnp[:, 0, h0], op=ALU.mult), [te, lnp_tok])
        tg0 = DVE(lambda e: e.tensor_tensor(out=x_res[:, s, h0], in0=x_res[:, s, h0], in1=lnp[:, 1, h0], op=ALU.add), [tf0])
        tf1 = POOL(lambda e: e.tensor_tensor(out=x_res[:, s, h1], in0=x_res[:, s, h1], in1=lnp[:, 0, h1], op=ALU.mult), [te, lnp_tok])
        tg1 = POOL(lambda e: e.tensor_tensor(out=x_res[:, s, h1], in0=x_res[:, s, h1], in1=lnp[:, 1, h1], op=ALU.add), [tf1])
        return {"x": [tg0, tg1], "h": [tg0, tg1]}

    def ln_partB(s, A, xT_dst):
        evs = []
        for half in range(2):
            k, bank, fr = PS.get()
            tk = None
            for c in range(4):
                kc = half * 4 + c
                tk = PE(lambda e, kc=kc, c=c, bank=bank: e.transpose(bank[:, c * 128:(c + 1) * 128], x_res[:, s, kc * 128:(kc + 1) * 128], ident),
                        [A["h"][half], fr], sig=(c == 3))
            ev = ACT(lambda e, half=half, bank=bank: e.activation(out=xT_dst[:, half * 4:half * 4 + 4, s * 128:(s + 1) * 128],
                                                                   in_=bank[:].rearrange("p (c t) -> p c t", t=128), func=AF.Copy), [tk])
            PS.rel(k, [ev])
            evs.append(ev)
        return evs

    def ln_epilogue(s, y_tok, lnp, lnp_tok, xT_dst, final_out_rows=None, want_x2T=False, x2T_free=()):
        A = ln_partA(s, y_tok, lnp, lnp_tok)
        res = {"x": A["x"]}
        if final_out_rows is not None:
            return res
        res["xT"] = ln_partB(s, A, xT_dst)
        return res

    def mixer(L, it, xT_src, xT_src_toks, xT_dst, lnp_k, lnp, lnp_tok, no_xT=False):
        seq_start = (it * TT) % SEQ_T == 0
        cs = it * 0
        W = w_in_d[L] if (it == 0 or stop is not None) else w_in_s[L]
        WO = w_out_d[L] if (it == 0 or stop is not None) else w_out_s[L]
        s_tok = None
        if seq_start:
            s_tok = [DVE(lambda e: e.memset(S[L][:], 0.0), [mix_state[L]["S_rd"]]),
                     DVE(lambda e: e.memset(S_bf[L][:], 0.0), [mix_state[L]["Sbf_rd"]])]
            mix_state[L]["S_w"] = s_tok[0]
            mix_state[L]["Sbf_w"] = s_tok[1]
        k0, w0, w0t = load_WA(W[:, 0:512], 512)
        k1, w1, w1t = load_WA(W[:, 512:1024], 512)
        k2, w2, w2t = load_WA(W[:, 1024:1536], 512)
        u_toks = []
        for c in range(4):
            k, bank, fr = PS.get()
            tk = mm_group(bank[:], [(w0[:, kc, c * 128:(c + 1) * 128], xT_src[:, kc, :]) for kc in range(KC)], [w0t, xT_src_toks, fr])
            ev = ACT(lambda e, c=c, bank=bank: e.activation(out=uT[:, c, :], in_=bank[:], func=AF.Gelu), [tk])
            PS.rel(k, [ev])
            u_toks.append(ev)
        WA.rel(k0, [tk])
        vln_toks = []
        agg = []
        for s in range(4):
            k, bank, fr = PS.get()
            tk = mm_group(bank[:], [(xT_src[:, kc, s * 128:(s + 1) * 128], w1[:, kc, 0:512]) for kc in range(KC)], [w1t, fr])
            ev = ACT(lambda e, bank=bank, s=s: e.activation(out=vg4[:, s, :], in_=bank[:], func=AF.Gelu), [tk, mix_state[L].get("vg_rd")])
            PS.rel(k, [ev])
            ts = [DVE(lambda e, h=h, s=s: e.bn_stats(out=st6[:, s * 4 + h, :], in_=vg4[:, s, h * 128:(h + 1) * 128]), [ev]) for h in range(4)]
            agg += [DVE(lambda e, h=h, s=s: e.bn_aggr(out=mv[:, s * 4 + h, :], in_=st6[:, s * 4 + h, :]), [ts[h]]) for h in range(4)]
        WA.rel(k1, [tk])
        k3, w3, w3t = load_WA(W[:, 1536:2048], 512)
        qk_toks = []
        for which, dst in ((0, qT), (1, kT)):
            for h in range(4):
                k, bank, fr = PS.get()
                c0 = which * 256 + h * 64
                tk = mm_group(bank[0:64, :], [(w2[:, kc, c0:c0 + 64], xT_src[:, kc, :]) for kc in range(KC)], [w2t, fr, mix_state[L]["qk_rd"]])
                ev = ACT(lambda e, dst=dst, h=h, bank=bank: e.activation(out=dst[:, h, :], in_=bank[0:64, :], func=AF.Copy), [tk])
                PS.rel(k, [ev])
                qk_toks.append(ev)
        ktok_toks = []
        for s in range(4):
            k, bank, fr = PS.get()
            tk = mm_group(bank[:, 0:256], [(xT_src[:, kc, s * 128:(s + 1) * 128], w2[:, kc, 256:512]) for kc in range(KC)], [fr])
            ev = ACT(lambda e, s=s, bank=bank: e.activation(out=k_tok[:, s, :], in_=bank[:, 0:256], func=AF.Copy), [tk])
            PS.rel(k, [ev])
            ktok_toks.append(ev)
        WA.rel(k2, [tk])
        k4, w4, w4t = load_WA(W[:, 2048:2576], 528)
        vtok_toks = []
        for s in range(4):
            k, bank, fr = PS.get()
            tk = mm_group(bank[:], [(xT_src[:, kc, s * 128:(s + 1) * 128], w3[:, kc, 0:512]) for kc in range(KC)], [w3t, fr])
            ev = ACT(lambda e, s=s, bank=bank: e.activation(out=v_tok[:, s, :], in_=bank[:], func=AF.Copy), [tk])
            PS.rel(k, [ev])
            vtok_toks.append(ev)
        WA.rel(k3, [tk])
        t_lv = ACT(lambda e: e.activation(out=lnv16[:], in_=mv[:, :, 1], func=AF.Ln, bias=eps_ln[:, 0:1]), [agg, eps_tok])
        t_rs = ACT(lambda e: e.activation(out=rstd16[:], in_=lnv16[:], func=AF.Exp, scale=-0.5), [t_lv])
        last_vhat_rd = None
        for s in range(4):
            tn = [DVE(lambda e, h=h, s=s: e.tensor_scalar(out=vhat[:, h * 128:(h + 1) * 128], in0=vg4[:, s, h * 128:(h + 1) * 128],
                                                           scalar1=mv[:, s * 4 + h, 0:1], scalar2=rstd16[:, s * 4 + h:s * 4 + h + 1], op0=ALU.subtract, op1=ALU.mult),
                      [t_rs, last_vhat_rd]) for h in range(4)]
            tg_ = POOL(lambda e: e.tensor_tensor(out=vhat[:], in0=vhat[:], in1=lnG[L][:], op=ALU.mult), [tn, setup_toks])
            tb_ = POOL(lambda e, s=s: e.tensor_tensor(out=vln[:, s, :], in0=vhat[:], in1=lnBt[L][:], op=ALU.add), [tg_])
            last_vhat_rd = tb_
            vln_toks.append(tb_)
        mix_state[L]["vg_rd"] = tn[-1]
        k5, wo0, wo0t = load_WA(WO[:, 0:512], 512)
        k, bank, fr = PS.get()
        tk = mm_group(bank[0:16, :], [(w4[:, kc, 512:528], xT_src[:, kc, :]) for kc in range(KC)], [w4t, fr])
        a_tok = ACT(lambda e, bank=bank: e.activation(out=a_aug[0:16, :], in_=bank[0:16, :], func=AF.Copy), [tk, a_ones_tok])
        PS.rel(k, [a_tok])
        sg_toks = []
        for c in range(4):
            k, bank, fr = PS.get()
            tk = mm_group(bank[:], [(w4[:, kc, c * 128:(c + 1) * 128], xT_src[:, kc, :]) for kc in range(KC)], [fr])
            ev = ACT(lambda e, c=c, bank=bank: e.activation(out=sgT[:, c, :], in_=bank[:], func=AF.Silu), [tk])
            PS.rel(k, [ev])
            ev2 = DVE(lambda e, c=c: e.tensor_scalar(out=sgT[:, c, :], in0=sgT[:, c, :], scalar1=gn[L][:, c:c + 1], scalar2=None, op0=ALU.mult),
                      [ev, setup_toks])
            sg_toks.append(ev2)
        WA.rel(k4, [tk])
        k6, wo1, wo1t = load_WA(WO[:, 512:1024], 512)
        issue_pre(8)
        mix_state[L]["xT_rd"] = tk

        tm = mix_state[L]
        out_toks = []
        pendB = []
        pendA1 = []
        pendA2 = []

        def gate_head(s):
            sc = slice(s * 128, (s + 1) * 128)
            k, bank, fr = PS.get()
            tz = PE(lambda e, sc=sc, bank=bank: e.matmul(bank[:, 0:256], a_aug[0:17, sc], wa2[L][0:17, :], start=True, stop=True),
                    [a_tok, setup_toks, fr], sig=True)
            t_e1 = ACT(lambda e, bank=bank: e.activation(out=e1[:], in_=bank[:, 0:256], func=AF.Exp, scale=-1.0), [tz, tm.get("e1_rd")])
            PS.rel(k, [t_e1])
            t_L = ACT(lambda e: e.activation(out=Lsp[:], in_=e1[:], func=AF.Ln, bias=one_t[:, 0:1]), [t_e1, tm.get("L_rd"), eps_tok])
            tm["e1_rd"] = t_L
            tm["t_L"] = t_L

        for s in range(4):
            sc = slice(s * 128, (s + 1) * 128)
            if s == 0:
                gate_head(0)
            t_L = tm["t_L"]
            k, bcum, fr = PS.get()
            tcum = None
            for h in range(4):
                tcum = PE(lambda e, h=h, bcum=bcum: e.matmul(bcum[0:64, h * 128:(h + 1) * 128], Lsp[:, h * 64:(h + 1) * 64], tri, start=True, stop=True),
                          [t_L, fr], sig=(h == 3))
            k2_, brem, fr2 = PS.get()
            trem = PE(lambda e, brem=brem: e.matmul(brem[:, 0:256], triu, Lsp[:], start=True, stop=True), [fr2], sig=True)
            tm["L_rd"] = trem
            t_Eq = ACT(lambda e, bcum=bcum: e.activation(out=Eq[:].rearrange("p h t -> p (h t)"), in_=bcum[0:64, :], func=AF.Exp, scale=-1.0 / 16.0),
                       [tcum, tm.get("Eq_rd")])
            t_Ek = ACT(lambda e, bcum=bcum: e.activation(out=Ek[:].rearrange("p h t -> p (h t)"), in_=bcum[0:64, :], func=AF.Exp, scale=1.0 / 16.0),
                       [tcum, tm.get("Ek_rd")])
            PS.rel(k, [t_Eq, t_Ek])
            t_Er = ACT(lambda e, brem=brem: e.activation(out=Er[:], in_=brem[:, 0:256], func=AF.Exp, scale=-1.0 / 16.0), [trem, tm.get("Er_rd")])
            PS.rel(k2_, [t_Er])
            t_qd = DVE(lambda e, sc=sc: e.scalar_tensor_tensor(out=qd[:], in0=qT[:, :, sc], scalar=0.125, in1=Eq[:], op0=ALU.mult, op1=ALU.mult),
                       [t_Eq, qk_toks, tm.get("qd_rd")])
            t_ki = DVE(lambda e, sc=sc: e.tensor_tensor(out=kinv[:], in0=kT[:, :, sc], in1=Ek[:], op=ALU.mult), [t_Ek, qk_toks, tm.get("kinv_rd")])
            tm["Ek_rd"] = t_ki
            t_kd = DVE(lambda e, s=s: e.tensor_tensor(out=kdec[:], in0=k_tok[:, s, :], in1=Er[:], op=ALU.mult), [t_Er, ktok_toks[s], tm.get("kdec_rd")])
            tm["Er_rd"] = t_kd
            while pendA1:
                pendA1.pop(0)()
            k, bsc, fr = PS.get()
            tsc = None
            for h in range(4):
                tsc = PE(lambda e, h=h, bsc=bsc: e.matmul(bsc[:, h * 128:(h + 1) * 128], kinv[:, h, :], qd[:, h, :], start=True, stop=True),
                         [t_qd, t_ki, fr], sig=(h == 3))
            tm["kinv_rd"] = tsc
            t_scm = DVE(lambda e, bsc=bsc: e.tensor_tensor(out=scm[:].rearrange("p h t -> p (h t)"), in0=bsc[:], in1=tri4, op=ALU.mult),
                        [tsc, tm.get("scm_rd")])
            PS.rel(k, [t_scm])
            k, bank, fr = PS.get()
            tk = None
            for h in range(4):
                tk = PE(lambda e, h=h, s=s, bank=bank: e.matmul(bank[:, h * 128:(h + 1) * 128], vln[:, s, h * 128:(h + 1) * 128], wmT[L][:, h, :],
                                                                start=True, stop=True), [vln_toks[s], setup_toks, fr], sig=(h == 3))
            k_gm, bank_gm, tk_gm = k, bank, tk
            t_sa = DVE(lambda e, bank_gm=bank_gm: e.tensor_tensor(out=tmp_sa[:], in0=bank_gm[:], in1=bB[L][:], op=ALU.add), [tk_gm, tm.get("tmp_sa_rd")])
            PS.rel(k_gm, [t_sa])
            t_oa = DVE(lambda e, sc=sc: e.tensor_tensor(out=catT[:, 0:4, sc], in0=tmp_sa[:].rearrange("p (h t) -> p h t", t=128),
                                                         in1=uT[:, 0:4, sc], op=ALU.mult), [t_sa, u_toks])
            tm["tmp_sa_rd"] = t_oa
            k, bo, fr = PS.get()
            to = None
            for c in range(2):
                cc = slice(c * 64, (c + 1) * 64)
                for h in range(4):
                    PE(lambda e, h=h, c=c, bo=bo, cc=cc: e.matmul(bo[:, h * 128 + c * 64:h * 128 + (c + 1) * 64], S_bf[L][:, h, :], qd[:, h, cc],
                                                                   start=True, stop=False), [tm.get("Sbf_w"), t_scm, fr], sig=False)
                    to = PE(lambda e, h=h, c=c, bo=bo, cc=cc, s=s: e.matmul(bo[:, h * 128 + c * 64:h * 128 + (c + 1) * 64], v_tok[:, s, h * 128:(h + 1) * 128],
                                                                            scm[:, h, cc], start=False, stop=True), [vtok_toks[s]], sig=(h == 3))
                tm["Sbf_rd"] = to
                k3_, bd, fr3 = PS.get()
                td_ = None
                for h in range(4):
                    td_ = PE(lambda e, h=h, bd=bd, cc=cc, s=s: e.matmul(bd[0:64, h * 128:(h + 1) * 128], kdec[cc, h * 64:(h + 1) * 64],
                                                                        v_tok[cc, s, h * 128:(h + 1) * 128], start=True, stop=True), [t_kd, fr3], sig=(h == 3))
                tus = []
                for h in range(4):
                    tus.append(DVE(lambda e, h=h, bd=bd, c=c: e.scalar_tensor_tensor(out=S[L][:, h, :], in0=S[L][:, h, :], scalar=Eq[:, h, c * 64 + 63:c * 64 + 64],
                                                                                    in1=bd[0:64, h * 128:(h + 1) * 128], op0=ALU.mult, op1=ALU.add),
                                   [td_, t_Eq, tm.get("S_w")]))
                tm["S_w"] = tus[-1]
                PS.rel(k3_, tus)
                tcast = DVE(lambda e: e.tensor_copy(out=S_bf[L][:], in_=S[L][:]), [tus, tm.get("Sbf_rd")])
                tm["Sbf_w"] = tcast
                tm["S_rd"] = tcast
                if c == 0:
                    while pendA2:
                        pendA2.pop(0)()
            tm["qd_rd"] = to
            tm["kdec_rd"] = td_
            tm["scm_rd"] = to
            tm["Eq_rd"] = tus[-1]
            t_sq = ACT(lambda e, bo=bo: e.activation(out=sq_bf[:], in_=bo[:], func=AF.Square), [to, tm.get("sq_rd")])
            k4_, bss, fr4 = PS.get()
            tss = PE(lambda e, bss=bss: e.matmul(bss[:], ones_bf[:], sq_bf[:], start=True, stop=True), [t_sq, fr4, ones_bf_tok], sig=True)
            tm["sq_rd"] = tss
            t_r0 = ACT(lambda e, bss=bss: e.activation(out=rr_l[:], in_=bss[:], func=AF.Ln, bias=eps_rms[:, 0:1]), [tss, eps_tok])
            t_r = ACT(lambda e: e.activation(out=rr[:], in_=rr_l[:], func=AF.Exp, scale=-0.5), [t_r0, tm.get("rr_rd")])
            PS.rel(k4_, [t_r0])
            while pendB:
                pendB.pop(0)()
            if s + 1 < 4:
                gate_head(s + 1)
            t_t1 = DVE(lambda e, bo=bo: e.tensor_tensor(out=t1[:], in0=bo[:], in1=rr[:], op=ALU.mult), [t_r, tm.get("t1_rd")])
            PS.rel(k, [t_t1, t_sq])
            tm["rr_rd"] = t_t1
            t_ob = DVE(lambda e, sc=sc: e.tensor_tensor(out=catT[:, 4:8, sc], in0=t1[:].rearrange("p (h t) -> p h t", t=128), in1=sgT[:, 0:4, sc], op=ALU.mult),
                       [t_t1, sg_toks])
            tm["t1_rd"] = t_ob
            ys = []
            for half, (wo, wot) in enumerate(((wo0, wo0t), (wo1, wo1t))):
                k, bank, fr = PS.get()
                tk = mm_group(bank[:], [(catT[:, c, sc], wo[:, c, 0:512]) for c in range(8)], [t_oa, t_ob, wot, fr])
                ty = DVE(lambda e, half=half, bank=bank, s=s: e.scalar_tensor_tensor(out=x_res[:, s, half * 512:(half + 1) * 512],
                                                                                    in0=x_res[:, s, half * 512:(half + 1) * 512], scalar=ALPHA,
                                                                                    in1=bank[:], op0=ALU.mult, op1=ALU.add), [tk, st["xres"][s]])
                PS.rel(k, [ty])
                ys.append(ty)
            last_wo = tk
            r = {"x": None, "xT": []}
            out_toks.append(r)

            def A1(s=s, ys=ys, r=r):
                r["td"] = ln_A1(s, ys)

            def A2(s=s, r=r):
                A_ = ln_A2(s, r["td"], lnp, lnp_tok)
                r["x"] = A_["x"]
                st["xres"][s] = list(A_["x"])
                if not no_xT:
                    def flushB(s=s, A_=A_, r=r):
                        r["xT"] = ln_partB(s, A_, xT_dst)
                        st["xres"][s] = list(A_["x"]) + r["xT"]
                    pendB.append(flushB)
            pendA1.append(A1)
            pendA2.append(A2)
        while pendA1:
            pendA1.pop(0)()
        while pendA2:
            pendA2.pop(0)()
        while pendB:
            pendB.pop(0)()
        WA.rel(k5, [last_wo])
        WA.rel(k6, [last_wo])
        tm["qk_rd"] = tm["qd_rd"]
        return out_toks

    def ffn(xT_src, xT_toks, wsrc, F, evac_fn, mid_hook=None, moe_q=None):
        nfc = F // 128
        hook["n"] = 0
        groups = []
        c0 = 0
        while c0 < F:
            gw = min(512, F - c0)
            groups.append((c0, gw))
            c0 += gw
        h_toks = []
        tk = None
        wb_plan = []
        c_ = 0
        while c_ < nfc:
            n_ = min(4, nfc - c_)
            wb_plan.append((c_, n_))
            c_ += n_
        wb_loaded = {}

        def issue_wb(i):
            if moe_q is not None:
                return
            if i < len(wb_plan) and i not in wb_loaded:
                cc_, nn_ = wb_plan[i]
                wb_loaded[i] = load_WB(wsrc('d', cc_ * 128, (cc_ + nn_) * 128), nn_)
        for gi_, (c0, gw) in enumerate(groups):
            if moe_q is not None:
                kg, wg, wgt = moe_q.pop('g')
                ku, wu, wut = moe_q.pop('u')
                wg = wg.rearrange("p (k c) -> p k c", c=512)
                wu = wu.rearrange("p (k c) -> p k c", c=512)
            else:
                kg, wg, wgt = load_WA(wsrc('g', c0, c0 + gw), gw)
                ku, wu, wut = load_WA(wsrc('u', c0, c0 + gw), gw)
            for j in range(gw // 128):
                fc = c0 // 128 + j
                k1_, bg, fr1 = PS.get()
                tg = mm_group(bg[:], [(wg[:, kc, j * 128:(j + 1) * 128], xT_src[:, kc, :]) for kc in range(KC)], [wgt, xT_toks, fr1])
                k2_, bu, fr2 = PS.get()
                tu = mm_group(bu[:], [(wu[:, kc, j * 128:(j + 1) * 128], xT_src[:, kc, :]) for kc in range(KC)], [wut, fr2])
                ks, sgt, frs = sg_ring.get()
                ta = ACT(lambda e, bg=bg, sgt=sgt: e.activation(out=sgt[:], in_=bg[:], func=AF.Silu), [tg, frs])
                PS.rel(k1_, [ta])
                th = DVE(lambda e, bu=bu, sgt=sgt, fc=fc: e.tensor_tensor(out=hT[:, fc, :], in0=bu[:], in1=sgt[:], op=ALU.mult), [tu, ta])
                PS.rel(k2_, [th])
                sg_ring.rel(ks, [th])
                h_toks.append(th)
            if moe_q is not None:
                moe_q.rel(kg, [tu])
                moe_q.rel(ku, [tu])
                if gi_ == 1:
                    for f in hook["deferred"]:
                        f()
                    hook["deferred"] = []
                continue
            cur_ring["WA"].rel(kg, [tu])
            cur_ring["WA"].rel(ku, [tu])
            if gi_ == min(2, len(groups) - 1):
                issue_wb(0)
                issue_wb(1)
        banks = [PS.get() for _ in range(8)]
        last = None
        for wi_, (c, n) in enumerate(wb_plan):
            issue_wb(wi_)
            if wi_ == 3 and mid_hook is not None:
                mid_hook()
            if moe_q is not None:
                kd, wd, wdt = moe_q.pop('d')
                wd = wd.rearrange("p (k c) -> p k c", c=1024)
            else:
                kd, wd, wdt = wb_loaded[wi_]
            for j in range(n):
                fc = c + j
                for s in range(4):
                    for half in range(2):
                        kb, bank, fr = banks[s * 2 + half]
                        last = PE(lambda e, bank=bank, fc=fc, s=s, half=half, wd=wd, j=j: e.matmul(bank[:], hT[:, fc, s * 128:(s + 1) * 128],
                                                                                                 wd[:, j, half * 512:(half + 1) * 512],
                                                                                                 start=(fc == 0), stop=(fc == nfc - 1)),
                                  [wdt, h_toks[fc], fr], sig=(fc == nfc - 1) or (j == n - 1 and s == 3 and half == 1))
                        if fc == nfc - 1:
                            banks[s * 2 + half] = (kb, bank, last)
            if moe_q is not None:
                moe_q.rel(kd, [last])
            else:
                WB.rel(kd, [last])
        res = []
        for s in range(4):
            for half in range(2):
                kb, bank, tok = banks[s * 2 + half]
                tv = evac_fn(s, half, bank, tok)
                PS.rel(kb, [tv])
                res.append(tv)
        return res

    mix_state = [dict(S_rd=None, Sbf_rd=None, qk_rd=None) for _ in range(DEPTH)]
    a_ones_tok = POOL(lambda e: e.memset(a_aug[:], 1.0), [])
    eps_tok = [POOL(lambda e: e.memset(eps_ln[:], LN_EPS), []), POOL(lambda e: e.memset(eps_rms[:], 128.0 * RMS_EPS), []), POOL(lambda e: e.memset(one_t[:], 1.0), [])]
    wdesc_tok = DVE(lambda e: e.tensor_scalar(out=wdesc[:], in0=consts[:, C_JROW:C_JROW + NE], scalar1=-1.0, scalar2=float(NE), op0=ALU.mult, op1=ALU.add), [c_tok])
    s_x = P.slot("s_x")
    s_o = P.slot("s_out")
    out_slots = s_o

    def dump(it):
        for s in range(4):
            P.dma("sp", out_d[it * TT + s * 128: it * TT + (s + 1) * 128, :], x_res[:, s, :], s_o, waits=[st["xres"][s]])

    NSUB = NT // 128
    NSLOT = (2 * NT + 8 * (TT - 1)) // TT
    dk = dict(kind="ExternalOutput") if debug else {}
    x2_d = nc.dram_tensor("x2_scr", [NT + 128, D], F32, **dk).ap()
    y_d = nc.dram_tensor("y_scr", [2 * NT + NSLOT * TT, D], F32, **dk).ap()
    sinfo_d = nc.dram_tensor("sinfo_scr", [NSLOT * TT, 4], I32, **dk).ap()
    sel_all = sb("sel_all", [128, NSUB, NE], F32)
    eq1_all = sb("eq1_all", [128, NSUB, NE], F32)
    gates_all = sb("gates_all", [128, NSUB, NE], F32)
    s_x2 = [P.slot(f"s_x2_{i}") for i in range(4)]
    s_x2z = P.slot("s_x2z")
    router_toks = []
    NG7 = D_FFE // 512
    wg_s = nc.dram_tensor("wg_scr", [NE * NG7 * 128, 4096], BF16).ap()
    wu_s = nc.dram_tensor("wu_scr", [NE * NG7 * 128, 4096], BF16).ap()
    wd_s = nc.dram_tensor("wd_scr", [NE * NG7 * 128, 4096], BF16).ap()
    s_pre = P.slot("s_pre")
    pre_jobs = []
    for e_ in range(NE):
        for g_ in range(NG7):
            r0 = (e_ * NG7 + g_) * 128
            pre_jobs.append((wg_s[r0:r0 + 128, :].rearrange("p (k c) -> p k c", c=512), eg_d[0, e_][:, g_ * 512:(g_ + 1) * 512].rearrange("(kc p) c -> p kc c", p=128)))
            pre_jobs.append((wu_s[r0:r0 + 128, :].rearrange("p (k c) -> p k c", c=512), eu_d[0, e_][:, g_ * 512:(g_ + 1) * 512].rearrange("(kc p) c -> p kc c", p=128)))
            pre_jobs.append((wd_s[r0:r0 + 128, :].rearrange("p (k c) -> p k c", c=1024), ed_d[0, e_][g_ * 512:(g_ + 1) * 512, :].rearrange("(c p) d -> p c d", p=128)))
    pre_per_tile = (len(pre_jobs) + max(ntiles - 1, 1) - 1) // max(ntiles - 1, 1)

    def dense_w(kind, a, b):
        src = (fg_d[0], fu_d[0], fd_d[0]) if hook["it"] == 0 else (fg_s, fu_s, fd_s)
        if kind == "g":
            return src[0][:, a:b]
        if kind == "u":
            return src[1][:, a:b]
        return src[2][a:b, :]

    for it in range(ntiles):
        for s in range(4):
            P.dma("sp", x_res[:, s, :], x_d[it * TT + s * 128: it * TT + (s + 1) * 128, :], s_x, waits=[st["xres"][s]])
        xtok = s_x.tok()
        xT_toks = []
        for s in range(4):
            for half in range(2):
                k, bank, fr = PS.get()
                tk = None
                for c in range(4):
                    kc = half * 4 + c
                    tk = PE(lambda e, kc=kc, c=c, bank=bank, s=s: e.transpose(bank[:, c * 128:(c + 1) * 128], x_res[:, s, kc * 128:(kc + 1) * 128], ident),
                            [xtok, c_tok, fr], sig=(c == 3))
                ev = ACT(lambda e, half=half, bank=bank, s=s: e.activation(out=xT[0][:, half * 4:half * 4 + 4, s * 128:(s + 1) * 128],
                                                                            in_=bank[:].rearrange("p (c t) -> p c t", t=128), func=AF.Copy), [tk])
                PS.rel(k, [ev])
                xT_toks.append(ev)
            st["xres"][s] = [xtok, tk]
        cur = 0
        stopped = False
        hook["it"] = it if stop is None else 0
        if it == 0 and stop is None:
            hook["pre"] = (cast_jobs, {"left": len(cast_jobs)})
            hook["pre_slot"] = s_cast
        else:
            hook["pre"] = (pre_jobs, {"left": pre_per_tile})
            hook["pre_slot"] = s_pre
        for L in range(DEPTH):
            is_moe = (L % 2 == 1)
            lk, lnp, lnp_tok = load_lnp(ln_mix_g_d[L], ln_mix_b_d[L])
            r = mixer(L, it, xT[cur], xT_toks, xT[1 - cur], lk, lnp, lnp_tok, no_xT=(is_moe and stop is None))
            lnp_ring.rel(lk, [r[-1]["x"]])
            cur = 1 - cur
            xT_toks = [t for rr_ in r for t in rr_["xT"]]
            if stop == ("mix", L):
                stopped = True
                break
            if not is_moe:
                lk, lnp, lnp_tok = load_lnp(ln_ffn_g_d[L], ln_ffn_b_d[L])

                def evac(s, half, bank, tok):
                    return DVE(lambda e: e.scalar_tensor_tensor(out=x_res[:, s, half * 512:(half + 1) * 512], in0=x_res[:, s, half * 512:(half + 1) * 512],
                                                                 scalar=ALPHA, in1=bank[:], op0=ALU.mult, op1=ALU.add), [tok, st["xres"][s]])
                ys = ffn(xT[cur], xT_toks, dense_w, D_FF, evac)
                rs = []
                As = [ln_partA(s, [ys[s * 2], ys[s * 2 + 1]], lnp, lnp_tok) for s in range(4)]
                for s in range(4):
                    r = {"x": As[s]["x"], "xT": ln_partB(s, As[s], xT[1 - cur])}
                    st["xres"][s] = list(r["x"]) + r["xT"]
                    rs.append(r)
                lnp_ring.rel(lk, [rs[-1]["x"]])
                cur = 1 - cur
                xT_toks = [t for rr_ in rs for t in rr_["xT"]]
                if stop == ("ffn", L):
                    stopped = True
                    break
            else:
                x2T_free = None
                prev_t10 = None
                for s in range(4):
                    g = it * 4 + s
                    evs = []
                    for half in range(2):
                        k, bank, fr = PS.get()
                        tk = None
                        for c in range(4):
                            kc = half * 4 + c
                            tk = PE(lambda e, kc=kc, c=c, bank=bank, s=s: e.transpose(bank[:, c * 128:(c + 1) * 128], x_res[:, s, kc * 128:(kc + 1) * 128], ident),
                                    [st["xres"][s], fr], sig=(c == 3))
                        ev = ACT(lambda e, half=half, bank=bank: e.activation(out=x2T[:, half * 4:half * 4 + 4, :],
                                                                               in_=bank[:].rearrange("p (c t) -> p c t", t=128), func=AF.Copy), [tk, x2T_free])
                        PS.rel(k, [ev])
                        evs.append(ev)
                    k, bank, fr = PS.get()
                    tl = mm_group(bank[:, 0:NE], [(x2T[:, kc, :], rw[:, kc, :]) for kc in range(KC)], [evs, c_tok, fr])
                    x2T_free = tl
                    t0 = DVE(lambda e, bank=bank: e.tensor_copy(out=lg[:], in_=bank[:, 0:NE]), [tl, prev_t10])
                    PS.rel(k, [t0])
                    t1_ = DVE(lambda e: e.reduce_max(out=m1[:], in_=lg[:], axis=AX.X), [t0])
                    t2a = DVE(lambda e: e.tensor_scalar(out=eq1[:], in0=lg[:], scalar1=m1[:, 0:1], scalar2=None, op0=ALU.is_ge), [t1_])
                    t2b = DVE(lambda e: e.tensor_tensor(out=eq1[:], in0=eq1[:], in1=wdesc[:], op=ALU.mult), [t2a, wdesc_tok])
                    t2c = DVE(lambda e: e.reduce_max(out=mt[:], in_=eq1[:], axis=AX.X), [t2b])
                    t2_ = DVE(lambda e, g=g: e.tensor_scalar(out=eq1_all[:, g, :], in0=eq1[:], scalar1=mt[:, 0:1], scalar2=None, op0=ALU.is_ge), [t2c])
                    t3_ = DVE(lambda e, g=g: e.scalar_tensor_tensor(out=l2[:], in0=eq1_all[:, g, :], scalar=-1e30, in1=lg[:], op0=ALU.mult, op1=ALU.add), [t2_])
                    t4_ = DVE(lambda e: e.reduce_max(out=m2[:], in_=l2[:], axis=AX.X), [t3_])
                    t5a = DVE(lambda e: e.tensor_scalar(out=sel[:], in0=l2[:], scalar1=m2[:, 0:1], scalar2=None, op0=ALU.is_ge), [t4_])
                    t5b = DVE(lambda e: e.tensor_tensor(out=sel[:], in0=sel[:], in1=wdesc[:], op=ALU.mult), [t5a])
                    t5c = DVE(lambda e: e.reduce_max(out=mt[:], in_=sel[:], axis=AX.X), [t5b])
                    t5d = DVE(lambda e: e.tensor_scalar(out=sel[:], in0=sel[:], scalar1=mt[:, 0:1], scalar2=None, op0=ALU.is_ge), [t5c])
                    t5_ = DVE(lambda e, g=g: e.tensor_tensor(out=sel_all[:, g, :], in0=sel[:], in1=eq1_all[:, g, :], op=ALU.add), [t5d])
                    t6_ = DVE(lambda e: e.tensor_scalar(out=nm1[:], in0=m1[:], scalar1=-1.0, scalar2=None, op0=ALU.mult), [t1_])
                    t7_ = ACT(lambda e: e.activation(out=ex[:], in_=lg[:], func=AF.Exp, bias=nm1[:, 0:1], scale=1.0), [t6_, t0])
                    t8_ = ACT(lambda e: e.activation(out=e2[:], in_=m2[:], func=AF.Exp, bias=nm1[:, 0:1], scale=1.0), [t6_, t4_])
                    t9a = DVE(lambda e: e.tensor_scalar(out=e2[:], in0=e2[:], scalar1=1.0, scalar2=None, op0=ALU.add), [t8_])
                    t9_ = DVE(lambda e: e.reciprocal(out=rden[:], in_=e2[:]), [t9a])
                    t10 = DVE(lambda e, g=g: e.scalar_tensor_tensor(out=gates_all[:, g, :], in0=ex[:], scalar=rden[:, 0:1], in1=sel_all[:, g, :], op0=ALU.mult, op1=ALU.mult),
                              [t9_, t7_, t5_])
                    prev_t10 = t10
                    router_toks.append(t10)
                    P.dma("sp", x2_d[g * 128:(g + 1) * 128, :], x_res[:, s, :], s_x2[s], waits=[st["xres"][s]])
                    st["xres"][s] = [s_x2[s].tok(), tk]
        if stopped:
            for s in range(4):
                P.dma("sp", out_d[it * TT + s * 128: it * TT + (s + 1) * 128, :], x_res[:, s, :], s_o, waits=[st["xres"][s]])
            out_tok_now = s_o.tok()
            for s in range(4):
                st["xres"][s] = [out_tok_now]

    hook["pre"] = None
    hook["it"] = 0
    if stop is None:
        while cast_jobs:
            o_, i_ = cast_jobs.pop(0)
            P.dma("pool", o_, i_, s_cast)
        while pre_jobs:
            o_, i_ = pre_jobs.pop(0)
            P.dma("pool", o_, i_, s_pre)
        W8 = NSUB * NE
        for e_ in ("dve", "pool", "act", "pe"):
            pt = [(P.sems[q], P.cnt[q]) for q in ("pe", "act", "dve", "pool")]
            P.op(e_, lambda e: e.nop(), pt, sig=False)
        mf_off = [512]

        def mf(shape, dt=F32):
            n = int(np.prod(shape))
            ap = mixF[:, mf_off[0]:mf_off[0] + n]
            mf_off[0] += n
            assert mf_off[0] <= 6400
            if dt is not F32:
                ap = ap.bitcast(dt)
            if len(shape) == 2:
                return ap.rearrange("p (a b) -> p a b", b=shape[1])
            if len(shape) == 3:
                return ap.rearrange("p (a b c) -> p a b c", b=shape[1], c=shape[2])
            return ap
        R1s = mf([NSUB, NE]); Tots = mf([NSUB, NE]); cs = mf([NSUB, NE]); pos = mf([NSUB, NE]); prod = mf([NSUB, NE]); oh2 = mf([NSUB, NE])
        cnt = mf([NE]); nsl = mf([NE]); tmp8 = mf([NE]); base = mf([NE]); endv = mf([NE]); posb = mf([NE])
        posk_f = mf([2, NSUB]); gatek = mf([2, NSUB]); posk_i = mf([2, NSUB], I32)
        info = mf([2, NSUB, 4], I32)
        eidf = mf([32]); tmpj = mf([32]); eid_i = mf([32], I32)
        padt = mf([NSLOT * 4, 4], I32)
        sinfo = [mf([4, 4], I32) for i in range(3)]
        sinfo_s = [P.slot(f"sinfo_s{i}") for i in range(3)]
        e1024 = mf([32])
        widx_g = mf([NSLOT, 8], I32)
        flat = lambda t: t[:].rearrange("p a b -> p (a b)")

        k, bank, fr = PS.get()
        tR = PE(lambda e, bank=bank: e.matmul(bank[:, 0:W8], slt, flat(sel_all), start=True, stop=True), [router_toks, c_tok, fr], sig=True)
        k2, bank2, fr2 = PS.get()
        tT = PE(lambda e, bank2=bank2: e.matmul(bank2[:, 0:W8], ones, flat(sel_all), start=True, stop=True), [fr2], sig=True)
        tR1 = DVE(lambda e, bank=bank: e.tensor_copy(out=flat(R1s), in_=bank[:, 0:W8]), [tR])
        PS.rel(k, [tR1])
        tTo = DVE(lambda e, bank2=bank2: e.tensor_copy(out=flat(Tots), in_=bank2[:, 0:W8]), [tT])
        PS.rel(k2, [tTo])
        tp_ = DVE(lambda e: e.memset(cs[:, 0, :], 0.0), [])
        for sub in range(1, NSUB):
            tp_ = DVE(lambda e, sub=sub: e.tensor_tensor(out=cs[:, sub, :], in0=cs[:, sub - 1, :], in1=Tots[:, sub - 1, :], op=ALU.add), [tp_, tTo])
        t_cnt = DVE(lambda e: e.tensor_tensor(out=cnt[:], in0=cs[:, NSUB - 1, :], in1=Tots[:, NSUB - 1, :], op=ALU.add), [tp_, tTo])
        tn_ = DVE(lambda e: e.memset(nsl[:], 0.0), [])
        for kk in range(8):
            ta_ = DVE(lambda e, kk=kk: e.tensor_scalar(out=tmp8[:], in0=cnt[:], scalar1=float(TT * kk) + 0.5, scalar2=None, op0=ALU.is_ge), [t_cnt, tn_])
            tn_ = DVE(lambda e: e.tensor_tensor(out=nsl[:], in0=nsl[:], in1=tmp8[:], op=ALU.add), [ta_])
        tb_ = DVE(lambda e: e.memset(base[:, 0:1], 0.0), [])
        for ee in range(1, NE):
            tb_ = DVE(lambda e, ee=ee: e.tensor_tensor(out=base[:, ee:ee + 1], in0=base[:, ee - 1:ee], in1=nsl[:, ee - 1:ee], op=ALU.add), [tb_, tn_])
        t_end = DVE(lambda e: e.tensor_tensor(out=endv[:], in0=base[:], in1=nsl[:], op=ALU.add), [tb_, tn_])
        t_pb = DVE(lambda e: e.tensor_scalar(out=posb[:], in0=base[:], scalar1=float(TT), scalar2=None, op0=ALU.mult), [tb_])
        t_tp = DVE(lambda e: e.tensor_tensor(out=flat(prod), in0=flat(cs), in1=flat(R1s), op=ALU.add), [tp_, tR1])
        t_pos = None
        tl_ = []
        for ee in range(NE):
            tl_.append(DVE(lambda e, ee=ee: e.tensor_scalar(out=pos[:, :, ee], in0=prod[:, :, ee], scalar1=posb[:, ee:ee + 1], scalar2=None, op0=ALU.add),
                           [t_tp, t_pb]))
        t_oh2 = DVE(lambda e: e.tensor_tensor(out=flat(oh2), in0=flat(sel_all), in1=flat(eq1_all), op=ALU.subtract), [router_toks])
        t_info0 = DVE(lambda e: e.memset(info[:].rearrange("p a b c -> p (a b c)"), 0), [])
        tprev = tl_[-1]
        info_toks = []
        for kk, oh in ((0, eq1_all), (1, oh2)):
            t_a = DVE(lambda e, oh=oh: e.tensor_tensor(out=flat(prod), in0=flat(oh), in1=flat(pos), op=ALU.mult), [tl_, t_oh2, tprev])
            t_b = DVE(lambda e, kk=kk: e.reduce_sum(out=posk_f[:, kk, :], in_=prod[:], axis=AX.X), [t_a])
            t_c = DVE(lambda e, oh=oh: e.tensor_tensor(out=flat(prod), in0=flat(oh), in1=flat(gates_all), op=ALU.mult), [t_b])
            t_d = DVE(lambda e, kk=kk: e.reduce_sum(out=gatek[:, kk, :], in_=prod[:], axis=AX.X), [t_c])
            tprev = t_d
            t_e = DVE(lambda e, kk=kk: e.tensor_copy(out=posk_i[:, kk, :], in_=posk_f[:, kk, :]), [t_b])
            t_f = DVE(lambda e, kk=kk: e.tensor_copy(out=info[:, kk, :, 0], in_=consts[:, C_TOK:C_TOK + NSUB]), [t_info0, c_tok])
            t_g = DVE(lambda e, kk=kk: e.tensor_scalar(out=info[:, kk, :, 1], in0=consts[:, C_TOK:C_TOK + NSUB], scalar1=float(kk * NT), scalar2=None, op0=ALU.add),
                      [t_info0, c_tok])
            t_h = DVE(lambda e, kk=kk: e.tensor_copy(out=info[:, kk, :, 2].bitcast(F32), in_=gatek[:, kk, :]), [t_d, t_info0])
            info_toks += [t_e, t_f, t_g, t_h]
        te_ = DVE(lambda e: e.memset(eidf[:], 0.0), [])
        for ee in range(NE):
            tj_ = DVE(lambda e, ee=ee: e.tensor_scalar(out=tmpj[:], in0=jrow, scalar1=endv[:, ee:ee + 1], scalar2=None, op0=ALU.is_ge), [t_end, te_, c_tok])
            te_ = DVE(lambda e: e.tensor_tensor(out=eidf[:], in0=eidf[:], in1=tmpj[:], op=ALU.add), [tj_])
        te_ = DVE(lambda e: e.tensor_scalar(out=eidf[:], in0=eidf[:], scalar1=float(NE - 1), scalar2=None, op0=ALU.min), [te_])
        t_eid = DVE(lambda e: e.tensor_copy(out=eid_i[:], in_=eidf[:]), [te_])
        t_e1 = DVE(lambda e: e.tensor_scalar(out=e1024[:], in0=eidf[:], scalar1=float(NG7 * 128), scalar2=None, op0=ALU.mult), [te_])
        widx_toks = []
        for j in range(NSLOT):
            widx_toks.append(DVE(lambda e, j=j: e.tensor_scalar(out=widx_g[:, j, 0:NG7], in0=consts[:, C_IG:C_IG + NG7], scalar1=e1024[:, j:j + 1], scalar2=None, op0=ALU.add),
                                 [t_e1, c_tok]))
        t_pad0 = DVE(lambda e: e.memset(padt[:].rearrange("p a b -> p (a b)"), 0), [])
        t_pad1 = DVE(lambda e: e.tensor_scalar(out=padt[:, :, 0], in0=consts[:, C_PADQ:C_PADQ + NSLOT * 4], scalar1=0.0, scalar2=float(NT), op0=ALU.mult, op1=ALU.add),
                     [t_pad0, c_tok])
        t_pad = DVE(lambda e: e.tensor_copy(out=padt[:, :, 1], in_=consts[:, C_PADQ:C_PADQ + NSLOT * 4]), [t_pad0, c_tok])
        s_pad = P.slot("s_pad")
        pad_tok = P.dma("sp", sinfo_d.rearrange("(p r) w -> p r w", p=128), padt[:], s_pad, waits=[t_pad, t_pad1])
        t_zr = DVE(lambda e: e.memset(vhat[:], 0.0), [mix_state[DEPTH - 1].get("vg_rd")])
        P.dma("sp", x2_d[NT:NT + 128, 0:512], vhat[:], s_x2z, waits=[t_zr])
        P.dma("sp", x2_d[NT:NT + 128, 512:1024], vhat[:], s_x2z, waits=[t_zr])
        s_sc = P.slot("s_scat")
        for kk in range(2):
            for sub in range(NSUB):
                P.raw("pool", lambda e, kk=kk, sub=sub: e.indirect_dma_start(out=sinfo_d, out_offset=bass.IndirectOffsetOnAxis(ap=posk_i[:, kk, sub:sub + 1], axis=0),
                                                                          in_=info[:, kk, sub, :], in_offset=None),
                      s_sc, waits=[pad_tok, info_toks])
        scat_tok = s_sc.tok()
        x2_all_fn = lambda: [q.tok() for q in s_x2] + [s_x2z.tok()]
        x2_all = None
        holder = {}
        s_g = P.slot("s_gather")
        s_y = P.slot("s_yscat")
        xg_free = [None]
        si_ring = Ring(sinfo)

        def prep_slot(j):
            kq, sit, frq = si_ring.get()
            tsi = P.dma("sp", sit[:], sinfo_d[j * TT:(j + 1) * TT, :].rearrange("(r p) w -> p r w", p=128), sinfo_s[kq], waits=[scat_tok, frq])
            tz = [xg_free[0]]
            if j == 0:
                tz = POOL(lambda e: e.memset(qk_buf[:, :], 0.0), [xg_free[0], mix_state[DEPTH - 1].get("qk_rd"), mix_state[DEPTH - 1].get("kinv_rd")])
            for r_ in range(4):
                P.raw("pool", lambda e, r_=r_, sit=sit: e.indirect_dma_start(out=xg4[:, r_, :], out_offset=None, in_=x2_d,
                                                                            in_offset=bass.IndirectOffsetOnAxis(ap=sit[:, r_, 0:1], axis=0)),
                      s_g, waits=[tsi, tz, x2_all_fn()])
            return kq, sit, tsi, s_g.tok()

        def transposes_slot(j, gtok):
            dst = xT[j % 2]
            toks = []
            tk = None
            for r_ in range(4):
                for half in range(2):
                    k, bank, fr = PS.get()
                    for c in range(4):
                        kc = half * 4 + c
                        tk = PE(lambda e, kc=kc, c=c, bank=bank, r_=r_: e.transpose(bank[:, c * 128:(c + 1) * 128], xg4[:, r_, kc * 128:(kc + 1) * 128], ident),
                                [gtok, fr], sig=(c == 3))
                    ev = ACT(lambda e, half=half, bank=bank, r_=r_, dst=dst: e.activation(out=dst[:, half * 4:half * 4 + 4, r_ * 128:(r_ + 1) * 128],
                                                                                         in_=bank[:].rearrange("p (c t) -> p c t", t=128), func=AF.Copy), [tk])
                    PS.rel(k, [ev])
                    toks.append(ev)
            xg_free[0] = tk
            return toks

        prep = {0: prep_slot(0)}
        cur_xT = transposes_slot(0, prep[0][3])
        hook["wa_pre"] = []
        hook["deferred"] = []

        class WQ:
            def __init__(self):
                self.tiles = [WAall[:, i * 4096:(i + 1) * 4096] for i in range(4)] + [WB.tiles[i][:].rearrange("p k c -> p (k c)") for i in range(2)]
                self.slots = list(WA_s) + list(WB_s)
                self.free = [list(widx_toks) + [s_pre.tok()] for _ in range(4)] + [list(f) + list(widx_toks) + [s_pre.tok()] for f in WB.free]
                self.seq = [(j, kind, gi) for j in range(NSLOT)
                            for (kind, gi) in ([(kk_, g_) for g_ in range(NG7) for kk_ in ("g", "u")] + [("d", g_) for g_ in range(NG7)])]
                self.pos = 0
                self.fifo = []
                for _ in range(6):
                    self.issue()

            def issue(self):
                if self.pos >= len(self.seq):
                    return
                j, kind, gi = self.seq[self.pos]
                k = self.pos % 6
                self.pos += 1
                src = {"g": wg_s, "u": wu_s, "d": wd_s}[kind]
                tile = self.tiles[k]
                idx = widx_g[:, j, gi:gi + 1]
                P.raw("pool", lambda e: e.indirect_dma_start(out=tile, out_offset=None, in_=src,
                                                             in_offset=bass.IndirectOffsetOnAxis(ap=idx, axis=0)), self.slots[k], waits=self.free[k])
                self.fifo.append((k, tile, self.slots[k].tok(), kind))

            def pop(self, kind):
                k, tile, tok, kd = self.fifo.pop(0)
                assert kd == kind, (kd, kind)
                return k, tile, tok

            def rel(self, k, toks):
                self.free[k] = [t for t in toks if t is not None]
                self.issue()

        wq = WQ()

        def moe_w_for(j):
            def moe_w(kind, a, b):
                gi_ = a // 512
                src = {"g": wg_s, "u": wu_s, "d": wd_s}[kind]
                return ("ind2", src, widx_g[:, j, gi_:gi_ + 1])
            return moe_w

        for j in range(NSLOT):
            kq, sit, tsi, _ = prep[j]
            if j + 1 < NSLOT:
                hook["deferred"].append(lambda j=j: prep.__setitem__(j + 1, prep_slot(j + 1)))
            moe_w = moe_w_for(j)

            def mid(j=j):
                if j + 1 < NSLOT:
                    w_ = moe_w_for(j + 1)
                    for (kind_, a_) in (("g", 0), ("u", 0), ("g", 512), ("u", 512)):
                        hook["wa_pre"].append(load_WA(w_(kind_, a_, a_ + 512), 512, _prefetch=True))

            def evac(s, half, bank, tok, sit=sit, tsi=tsi):
                return DVE(lambda e: e.tensor_scalar(out=x_res[:, s, half * 512:(half + 1) * 512], in0=bank[:], scalar1=sit[:, s, 2:3].bitcast(F32), scalar2=None,
                                                     op0=ALU.mult), [tok, tsi, st["xres"][s]])
            ys = ffn(xT[j % 2], cur_xT, moe_w, D_FFE, evac, moe_q=wq)
            if j + 1 < NSLOT:
                cur_xT = transposes_slot(j + 1, prep[j + 1][3])

            def do_scatter(ys=ys, sit=sit, kq=kq):
                for r_ in range(4):
                    P.raw("pool", lambda e, r_=r_: e.indirect_dma_start(out=y_d, out_offset=bass.IndirectOffsetOnAxis(ap=sit[:, r_, 1:2], axis=0),
                                                                      in_=x_res[:, r_, :], in_offset=None),
                          s_y, waits=[ys[r_ * 2], ys[r_ * 2 + 1]])
                tk_ = s_y.tok()
                for r_ in range(4):
                    st["xres"][r_] = [tk_]
                si_ring.rel(kq, [tk_])
            if j + 1 < NSLOT:
                hook["deferred"].append(do_scatter)
            else:
                do_scatter()
        y_all = s_y.tok()
        if debug:
            dbg_d = nc.dram_tensor("dbg", [128, 32 + NSLOT * 36 + 32 + 8 * 4], I32, kind="ExternalOutput").ap()
            s_dbg = P.slot("s_dbg")
            P.dma("sp", dbg_d[:, 0:32], eid_i[:], s_dbg, waits=[t_eid])
            P.dma("sp", dbg_d[:, 32:32 + NSLOT * 8], widx_g[:].rearrange("p a b -> p (a b)"), s_dbg, waits=[widx_toks])
            P.dma("sp", dbg_d[:, 32 + NSLOT * 36:32 + NSLOT * 36 + 32], eidf[:].bitcast(I32), s_dbg, waits=[t_eid])
            for q_, tt_ in enumerate((cnt, nsl, base, endv)):
                P.dma("sp", dbg_d[:, 64 + NSLOT * 36 + q_ * 8:64 + NSLOT * 36 + (q_ + 1) * 8], tt_[:].bitcast(I32), s_dbg, waits=[t_eid])
            dbg2_d = nc.dram_tensor("dbg2", [128, 4, 1024], F32, kind="ExternalOutput").ap()
            dbg3_d = nc.dram_tensor("dbg3", [128, 4, 1024], F32, kind="ExternalOutput").ap()
            dbg4_d = nc.dram_tensor("dbg4", [128, 8, 512], F32, kind="ExternalOutput").ap()
            P.dma("sp", dbg2_d, xg4[:], s_dbg, waits=[s_g.tok()])
            P.dma("sp", dbg3_d, x_res[:], s_dbg, waits=[st["xres"]])
            tq = None
            for kc_ in range(8):
                tq = ACT(lambda e, kc_=kc_: e.activation(out=vg4[:, 0, :], in_=xT[0][:, kc_, :], func=AF.Copy), [cur_xT, tq, s_dbg.tok()])
                P.dma("sp", dbg4_d[:, kc_, :], vg4[:, 0, :], s_dbg, waits=[tq])
                tq = s_dbg.tok()
            y_all = [y_all, s_dbg.tok()]
        lk, lnp, lnp_tok = load_lnp(ln_ffn_g_d[DEPTH - 1], ln_ffn_b_d[DEPTH - 1])
        ovlF = ovl[:, :].bitcast(F32)
        yb_ring = Ring([xg4[:, 0:2, :], xg4[:, 2:4, :], ovlF[:, 0:2048].rearrange("p (a b) -> p a b", b=1024),
                        ovlF[:, 2048:4096].rearrange("p (a b) -> p a b", b=1024)])
        s_f = [P.slot(f"s_fin{i}") for i in range(4)]
        s_yl = [P.slot(f"s_yl{i}") for i in range(4)]
        s_of = [P.slot(f"s_of{i}") for i in range(4)]
        last_x = None
        PF = 2

        def issue_loads(g):
            s = g % 4
            tx = P.dma("sp", x_res[:, s, :], x2_d[g * 128:(g + 1) * 128, :], s_f[s], waits=[st["xres"][s], y_all, x2_all_fn()])
            ky, yt_, fry = yb_ring.get()
            P.dma("sp", yt_[:, 0, :], y_d[g * 128:(g + 1) * 128, :], s_yl[ky], waits=[fry, y_all, xg_free[0]])
            ty = P.dma("sp", yt_[:, 1, :], y_d[NT + g * 128:NT + (g + 1) * 128, :], s_yl[ky], waits=[fry, y_all])
            return tx, ty, ky, yt_
        loads = {}
        for g in range(min(PF, NSUB)):
            loads[g] = issue_loads(g)
        identA = sb("identA", [128, 128], F32)
        t_idA = DVE(lambda e: e.tensor_scalar(out=identA[:], in0=ident, scalar1=ALPHA, scalar2=None, op0=ALU.mult), [c_tok])
        for g in range(NSUB):
            s = g % 4
            if g + PF < NSUB:
                loads[g + PF] = issue_loads(g + PF)
            tx, ty, ky, yt_ = loads.pop(g)
            cb = []
            for half in range(2):
                hs = slice(half * 512, (half + 1) * 512)
                kb, bank, fr = PS.get()
                PE(lambda e, bank=bank, s=s, hs=hs: e.matmul(bank[:], identA[:], x_res[:, s, hs], start=True, stop=False), [tx, t_idA, fr])
                PE(lambda e, bank=bank, yt_=yt_, hs=hs: e.matmul(bank[:], ident, yt_[:, 0, hs], start=False, stop=False), [ty])
                tkm = PE(lambda e, bank=bank, yt_=yt_, hs=hs: e.matmul(bank[:], ident, yt_[:, 1, hs], start=False, stop=True), [], sig=True)
                cb.append((kb, bank, tkm))
            yb_ring.rel(ky, [cb[1][2]])
            ta = DVE(lambda e, s=s, b0=cb[0][1]: e.bn_stats(out=ln_st4[:, s, 0, :], in_=b0[:]), [cb[0][2], ln_rd[s]])
            tb = DVE(lambda e, s=s, b1=cb[1][1]: e.bn_stats(out=ln_st4[:, s, 1, :], in_=b1[:]), [cb[1][2]])
            tc_ = DVE(lambda e, s=s: e.bn_aggr(out=ln_mv4[:, s, :], in_=ln_st4[:, s, :, :].rearrange("p a b -> p (a b)")), [ta, tb])
            td0 = ACT(lambda e, s=s: e.activation(out=ln_l4[:, s:s + 1], in_=ln_mv4[:, s, 1:2], func=AF.Ln, bias=eps_ln[:, 0:1]), [tc_, eps_tok])
            td = ACT(lambda e, s=s: e.activation(out=ln_r4[:, s:s + 1], in_=ln_l4[:, s:s + 1], func=AF.Exp, scale=-0.5), [td0])
            tes = []
            for half in range(2):
                hs = slice(half * 512, (half + 1) * 512)
                kb, bank, tkm = cb[half]
                te_ = DVE(lambda e, s=s, hs=hs, bank=bank: e.tensor_scalar(out=x_res[:, s, hs], in0=bank[:], scalar1=ln_mv4[:, s, 0:1], scalar2=ln_r4[:, s:s + 1],
                                                                        op0=ALU.subtract, op1=ALU.mult), [td, cb[1][2]])
                PS.rel(kb, [te_])
                tes.append(te_)
            ln_rd[s] = tes[1]
            h0, h1 = slice(0, 512), slice(512, 1024)
            tf0 = DVE(lambda e, s=s: e.tensor_tensor(out=x_res[:, s, h0], in0=x_res[:, s, h0], in1=lnp[:, 0, h0], op=ALU.mult), [tes[0], lnp_tok])
            tg0 = DVE(lambda e, s=s: e.tensor_tensor(out=x_res[:, s, h0], in0=x_res[:, s, h0], in1=lnp[:, 1, h0], op=ALU.add), [tf0])
            tf1 = POOL(lambda e, s=s: e.tensor_tensor(out=x_res[:, s, h1], in0=x_res[:, s, h1], in1=lnp[:, 0, h1], op=ALU.mult), [tes[1], lnp_tok])
            tg1 = POOL(lambda e, s=s: e.tensor_tensor(out=x_res[:, s, h1], in0=x_res[:, s, h1], in1=lnp[:, 1, h1], op=ALU.add), [tf1])
            last_x = [tg0, tg1]
            fo = P.dma("act", out_d[g * 128:(g + 1) * 128, :], x_res[:, s, :], s_of[s], waits=[tg0, tg1])
            st["xres"][s] = [fo]
        lnp_ring.rel(lk, [last_x])
        final_toks = [q.tok() for q in s_of]
    P.emit([s_o.tok()] + (final_toks if stop is None else []))
    return nc


_PARAM_NAMES = ["w_in", "gm_ws", "gm_bs", "gm_ln_g", "gm_ln_b", "gla_wa2", "gla_ba", "gla_norm_g", "w_out", "ln_mix_g", "ln_mix_b",
                "ffn_w_gate", "ffn_w_up", "ffn_w_down", "router_w", "exp_w_gate", "exp_w_up", "exp_w_down", "ln_ffn_g", "ln_ffn_b"]


def run(inputs, NT, SEQ_T, ncores, stop=None, debug=False):
    nc = build(NT, SEQ_T, stop, debug)
    x = np.ascontiguousarray(np.asarray(inputs["x"], dtype=np.float32)).reshape(-1, D)
    base = {}
    for n in _PARAM_NAMES:
        a = np.ascontiguousarray(np.asarray(inputs[n], dtype=np.float32))
        if n == "gm_bs":
            a = a.reshape(DEPTH, 512)
        base[n] = a
    base["consts"] = make_consts(NT)
    in_maps = []
    for c in range(ncores):
        m = dict(base)
        m["x"] = np.ascontiguousarray(x[c * NT:(c + 1) * NT])
        in_maps.append(m)
    res = run_bass_kernel_spmd(nc, in_maps, core_ids=list(range(ncores)))
    return np.concatenate([r["out"] for r in res.results], axis=0), res


def kernel(**inputs):
    x = np.asarray(inputs["x"])
    B, S_, _ = x.shape
    NT = (B * S_) // NCORES
    out, _ = run(inputs, NT, S_, NCORES)
    return out.reshape(B, S_, D).astype(np.float32)
```

```python
import numpy as np
import concourse.bass as bass
import concourse.mybir as mybir
from concourse.bass_utils import run_bass_kernel_spmd

F32 = mybir.dt.float32
BF16 = mybir.dt.bfloat16
AF = mybir.ActivationFunctionType
ALU = mybir.AluOpType
AX = mybir.AxisListType

D = 1024
KC = 8
DEPTH = 2
IN_COLS = 2576
D_FF = 2816
D_FFE = 3584
NE = 8
ALPHA = float((2 * DEPTH) ** 0.25)
LN_EPS = 1e-5
RMS_EPS = 1e-6
TT = 512
NCORES = 8

ENGS = ["pe", "act", "dve", "pool", "sp"]


class Slot:
    def __init__(self, prog, name):
        self.sem = prog.nc.alloc_semaphore(name)
        self.n = 0

    def tok(self):
        return (self.sem, self.n * 16)


class Prog:
    def __init__(self, nc):
        self.nc = nc
        self.ops = {e: [] for e in ENGS}
        self.sems = {e: nc.alloc_semaphore("sem_" + e) for e in ENGS}
        self.cnt = {e: 0 for e in ENGS}
        self.known = {e: {} for e in ENGS}
        self.nslots = 0

    def slot(self, name=None):
        self.nslots += 1
        return Slot(self, name or f"dslot{self.nslots}")

    def _flat(self, waits, acc):
        for t in waits:
            if t is None:
                continue
            if isinstance(t, (list, tuple)) and (len(t) == 0 or isinstance(t[0], (list, tuple)) or t[0] is None):
                self._flat(t, acc)
            else:
                acc[t[0]] = max(acc.get(t[0], 0), t[1])

    def _waits(self, eng, waits):
        w = {}
        self._flat(waits, w)
        out = []
        kn = self.known[eng]
        for s, v in w.items():
            if v <= 0 or kn.get(s, 0) >= v:
                continue
            kn[s] = v
            out.append((s, v))
        return out

    def op(self, eng, fn, waits=(), sig=True):
        w = self._waits(eng, waits)
        tok = None
        if sig:
            self.cnt[eng] += 1
            tok = (self.sems[eng], self.cnt[eng])
        self.ops[eng].append((w, fn, 1 if sig else 0, self.sems[eng]))
        return tok

    def dma(self, q, out, in_, slot, waits=(), **kw):
        w = self._waits(q, waits)
        slot.n += 1
        def fn(e):
            o = out(e) if callable(out) else out
            i = in_(e) if callable(in_) else in_
            try:
                return e.dma_start(out=o, in_=i, **kw)
            except Exception:
                print("DMA FAIL", q, o, i)
                raise
        self.ops[q].append((w, fn, 16, slot.sem))
        return slot.tok()

    def raw(self, q, fn, slot, waits=()):
        w = self._waits(q, waits)
        slot.n += 1
        self.ops[q].append((w, fn, 16, slot.sem))
        return slot.tok()

    def emit(self, final_waits):
        nc = self.nc
        engmap = {"pe": "tensor", "act": "scalar", "dve": "vector", "pool": "gpsimd", "sp": "sync"}
        with nc.Block() as block:
            for e in ENGS:
                ops = self.ops[e]
                extra = self._waits(e, final_waits) if e == "sp" else []

                def body(engine, ops=ops, extra=extra):
                    for (w, fn, inc, sem) in ops:
                        for (s, v) in w:
                            engine.wait_ge(s, v)
                        ins = fn(engine)
                        if inc:
                            ins.then_inc(sem, inc)
                    for (s, v) in extra:
                        engine.wait_ge(s, v)

                getattr(block, engmap[e])(body)


class Ring:
    def __init__(self, tiles):
        self.tiles = tiles
        self.n = len(tiles)
        self.free = [[] for _ in tiles]
        self.i = 0

    def get(self):
        k = self.i % self.n
        self.i += 1
        return k, self.tiles[k], self.free[k]

    def rel(self, k, toks):
        self.free[k] = [t for t in toks if t is not None]


def make_consts(NT=4096):
    j = np.arange(128)[:, None]
    i = np.arange(128)[None, :]
    same = (j // 64) == (i // 64)
    ident = np.eye(128, dtype=np.float32)
    tri = ((j <= i) & same).astype(np.float32)
    triu = ((j > i) & same).astype(np.float32)
    causal = (j <= i).astype(np.float32)
    ones = np.ones((128, 128), np.float32)
    slt = (j < i).astype(np.float32)
    jrow = np.broadcast_to(np.arange(32, dtype=np.float32)[None, :], (128, 32))
    tokidx = (np.arange(32)[None, :] * 128 + np.arange(128)[:, None]).astype(np.float32)
    p_ = np.arange(128)[:, None]
    iota_g = (np.arange(8)[None, :] * 128 + p_).astype(np.float32)
    iota_d = (np.arange(28)[None, :] * 128 + p_).astype(np.float32)
    nslot = (2 * NT + 8 * (TT - 1)) // TT
    padq = np.zeros((128, 96), np.float32)
    padq[:, :nslot * 4] = 2 * NT + (p_ * (nslot * 4) + np.arange(nslot * 4)[None, :])
    return np.ascontiguousarray(np.concatenate([ident, tri, tri, tri, tri, triu, causal, ones, slt, jrow, tokidx, iota_g, iota_d, padq], axis=1))


C_ID, C_TRI4, C_TRIU, C_CAUS, C_ONES, C_SLT, C_JROW, C_TOK, C_IG, C_IDN, C_PADQ, C_N = 0, 128, 640, 768, 896, 1024, 1152, 1184, 1216, 1224, 1252, 1348
I32 = mybir.dt.int32


def build(NT, SEQ_T, stop=None, debug=False):
    assert NT % TT == 0 and SEQ_T % TT == 0
    ntiles = NT // TT
    nc = bass.Bass("TRN2", target_bir_lowering=False)

    def din(name, shape):
        return nc.dram_tensor(name, list(shape), F32, kind="ExternalInput").ap()

    x_d = din("x", [NT, D])
    w_in_d = din("w_in", [DEPTH, D, IN_COLS])
    gm_ws_d = din("gm_ws", [DEPTH, 4, 128, 128])
    gm_bs_d = din("gm_bs", [DEPTH, 512])
    gm_ln_g_d = din("gm_ln_g", [DEPTH, 512])
    gm_ln_b_d = din("gm_ln_b", [DEPTH, 512])
    wa2_d = din("gla_wa2", [DEPTH, 16, 256])
    ba_d = din("gla_ba", [DEPTH, 256])
    gng_d = din("gla_norm_g", [DEPTH, 512])
    w_out_d = din("w_out", [DEPTH, D, D])
    ln_mix_g_d = din("ln_mix_g", [DEPTH, D])
    ln_mix_b_d = din("ln_mix_b", [DEPTH, D])
    fg_d = din("ffn_w_gate", [1, D, D_FF])
    fu_d = din("ffn_w_up", [1, D, D_FF])
    fd_d = din("ffn_w_down", [1, D_FF, D])
    rw_d = din("router_w", [1, D, NE])
    eg_d = din("exp_w_gate", [1, NE, D, D_FFE])
    eu_d = din("exp_w_up", [1, NE, D, D_FFE])
    ed_d = din("exp_w_down", [1, NE, D_FFE, D])
    ln_ffn_g_d = din("ln_ffn_g", [DEPTH, D])
    ln_ffn_b_d = din("ln_ffn_b", [DEPTH, D])
    consts_d = din("consts", [128, C_N])
    out_d = nc.dram_tensor("out", [NT, D], F32, kind="ExternalOutput").ap()

    P = Prog(nc)
    sb = nc.alloc_sbuf_tensor

    consts = sb("consts_sb", [128, C_N], F32)
    ident = consts[:, C_ID:C_ID + 128]
    tri4 = consts[:, C_TRI4:C_TRI4 + 512]
    tri = consts[:, C_TRI4:C_TRI4 + 128]
    triu = consts[:, C_TRIU:C_TRIU + 128]
    causal = consts[:, C_CAUS:C_CAUS + 128]
    ones = consts[:, C_ONES:C_ONES + 128]
    slt = consts[:, C_SLT:C_SLT + 128]
    jrow = consts[:, C_JROW:C_JROW + 32]

    wmT = [sb(f"wmT{L}", [128, 4, 128], BF16) for L in range(DEPTH)]
    bB = [sb(f"bB{L}", [128, 512], F32) for L in range(DEPTH)]
    lnG = [sb(f"lnG{L}", [128, 512], F32) for L in range(DEPTH)]
    lnBt = [sb(f"lnBt{L}", [128, 512], F32) for L in range(DEPTH)]
    wa2 = [sb(f"wa2{L}", [17, 256], F32) for L in range(DEPTH)]
    gn = [sb(f"gn{L}", [128, 4], F32) for L in range(DEPTH)]
    rw = sb("rw_sb", [128, KC, NE], F32)
    lnp_ring = Ring([sb(f"lnp{i}", [128, 2, D], F32) for i in range(1)])
    lnp_slots = [P.slot(f"lnp_s{i}") for i in range(1)]

    WAall = sb("WAall", [128, 4 * KC * 528], BF16)
    WA = Ring([WAall[:, i * KC * 528:(i + 1) * KC * 528].rearrange("p (k c) -> p k c", c=528) for i in range(4)])
    WA_s = [P.slot(f"WA_s{i}") for i in range(4)]
    WAm = Ring([WAall[:, i * 4096:(i + 1) * 4096].rearrange("p (k c) -> p k c", c=512) for i in range(4)])
    cur_ring = {"WA": WA}
    WB = Ring([sb(f"WB{i}", [128, 4, D], BF16) for i in range(2)])
    WB_s = [P.slot(f"WB_s{i}") for i in range(2)]

    x_res = sb("x_res", [128, 4, D], F32)
    xT = [sb(f"xT{i}", [128, KC, TT], BF16) for i in range(2)]
    ovl = sb("ovl", [128, 28 * TT], BF16)
    hT = ovl[:, :].rearrange("p (c t) -> p c t", t=TT)
    uT = ovl[:, 0:2048].rearrange("p (c t) -> p c t", t=TT)
    sgT = ovl[:, 2048:4096].rearrange("p (c t) -> p c t", t=TT)
    vln = ovl[:, 4096:6144].rearrange("p (c t) -> p c t", t=512)
    v_tok = ovl[:, 6144:8192].rearrange("p (c t) -> p c t", t=512)
    catT = ovl[:, 8192:12288].rearrange("p (c t) -> p c t", t=TT)
    qk_buf = sb("qk_buf", [128, 4096], F32)
    qT = qk_buf[0:64, 0:2048].rearrange("p (c t) -> p c t", t=TT)
    kT = qk_buf[0:64, 2048:4096].rearrange("p (c t) -> p c t", t=TT)
    xg4 = qk_buf[:, :].rearrange("p (c t) -> p c t", t=D)
    mixF = sb("mixF", [128, 6400], F32)
    k_tok = mixF[:, 4864:5888].rearrange("p (c t) -> p c t", t=256)
    a_aug = mixF[0:17, 5888:6400]
    S = [sb(f"S{L}", [64, 4, 128], F32) for L in range(DEPTH)]
    S_bf = [sb(f"Sbf{L}", [64, 4, 128], BF16) for L in range(DEPTH)]
    scrA = sb("scrA", [128, 2048], F32)
    vg4 = scrA[:, :].rearrange("p (c t) -> p c t", t=512)
    st6 = sb("st6", [128, 16, 6], F32)
    mv = sb("mv", [128, 16, 2], F32)
    rstd16 = sb("rstd16", [128, 16], F32)
    lnv16 = sb("lnv16", [128, 16], F32)
    eps_ln = sb("eps_ln", [128, 1], F32)
    eps_rms = sb("eps_rms", [128, 1], F32)
    one_t = sb("one_t", [128, 1], F32)
    ln_l = sb("ln_l", [128, 1], F32)
    rr_l = mixF[:, 2048:2560]
    vhat = mixF[:, 0:512]
    tmp_sa = mixF[:, 512:1024]
    e1 = mixF[:, 3072:3328]
    Lsp = mixF[:, 3328:3584]
    Eq = mixF[0:64, 3840:4352].rearrange("p (c t) -> p c t", t=128)
    Ek = mixF[0:64, 4352:4864].rearrange("p (c t) -> p c t", t=128)
    Er = mixF[:, 3584:3840]
    qd = sb("qd", [64, 4, 128], BF16)
    kinv = sb("kinv", [64, 4, 128], BF16)
    kdec = sb("kdec", [128, 256], BF16)
    scm = sb("scm", [128, 4, 128], BF16)
    ones_bf = sb("ones_bf", [128, 128], BF16)
    sq_bf = sb("sq_bf", [128, 512], BF16)
    sq = mixF[:, 1024:1536]
    rr = mixF[:, 1536:2048]
    t1 = mixF[:, 2560:3072]
    ln_st = sb("ln_st", [128, 2, 6], F32)
    ln_mv = sb("ln_mv", [128, 2], F32)
    ln_r = sb("ln_r", [128, 1], F32)
    sg_ring = Ring([sb(f"sg{i}", [128, TT], F32) for i in range(2)])
    x2T = scrA[:, 0:1024].rearrange("p (c t) -> p c t", t=128)
    lg = sb("lg", [128, NE], F32)
    m1 = sb("m1", [128, 1], F32)
    nm1 = sb("nm1", [128, 1], F32)
    m2 = sb("m2", [128, 1], F32)
    eq1 = sb("eq1", [128, NE], F32)
    l2 = sb("l2", [128, NE], F32)
    sel = sb("sel", [128, NE], F32)
    wdesc = sb("wdesc", [128, NE], F32)
    mt = sb("mt", [128, 1], F32)
    ex = sb("ex", [128, NE], F32)
    e2 = sb("e2", [128, 1], F32)
    rden = sb("rden", [128, 1], F32)
    gates = sb("gates", [128, 4, NE], F32)

    PS = Ring([nc.alloc_psum_tensor(f"ps{i}", [128, 512], F32) for i in range(8)])

    def PE(fn, waits=(), sig=False):
        return P.op("pe", fn, waits, sig)

    def ACT(fn, waits=()):
        return P.op("act", fn, waits, True)

    def DVE(fn, waits=()):
        return P.op("dve", fn, waits, True)

    def POOL(fn, waits=()):
        return P.op("pool", fn, waits, True)

    def mm_group(out, pairs, waits):
        n = len(pairs)
        tok = None
        for i, (l, r) in enumerate(pairs):
            tok = PE(lambda e, l=l, r=r, i=i: e.matmul(out, l, r, start=(i == 0), stop=(i == n - 1)),
                     waits if i == 0 else (), sig=(i == n - 1))
        return tok

    s_const = P.slot("s_const")
    P.dma("sp", consts[:], consts_d, s_const)
    gmw_raw = t1[:, :].rearrange("p (c t) -> p c t", t=128)
    gn_raw = sb("gn_raw", [128, 4], F32)
    setup_toks = []
    for L in range(DEPTH):
        P.dma("sp", bB[L][:], gm_bs_d[L].partition_broadcast(128), s_const)
        P.dma("sp", lnG[L][:], gm_ln_g_d[L].partition_broadcast(128), s_const)
        P.dma("sp", lnBt[L][:], gm_ln_b_d[L].partition_broadcast(128), s_const)
        P.dma("sp", wa2[L][0:16, :], wa2_d[L], s_const)
        P.dma("sp", wa2[L][16:17, :], ba_d[L:L + 1, :], s_const)
    P.dma("sp", rw[:], rw_d[0].rearrange("(kc p) e -> p kc e", p=128), s_const)
    c_tok = s_const.tok()
    caus4 = sq
    ones_bf_tok = POOL(lambda e: e.tensor_copy(out=ones_bf[:], in_=ones), [c_tok])
    cz = [POOL(lambda e, h=h: e.tensor_copy(out=caus4[:, h * 128:(h + 1) * 128], in_=causal), [c_tok]) for h in range(4)]
    prev = [c_tok]
    for L in range(DEPTH):
        s_l = P.slot(f"s_setup{L}")
        P.dma("sp", gmw_raw[:], gm_ws_d[L].rearrange("h t s -> t h s"), s_l, waits=prev)
        P.dma("sp", gn_raw[:], gng_d[L].rearrange("(h e) -> e h", e=128), s_l, waits=prev, allow_slow_non_contiguous=True)
        lt = s_l.tok()
        k, bank, fr = PS.get()
        tk = None
        for h in range(4):
            tk = PE(lambda e, h=h, bank=bank: e.transpose(bank[:, h * 128:(h + 1) * 128], gmw_raw[:, h, :], ident),
                    [lt, c_tok, fr], sig=(h == 3))
        t_a = DVE(lambda e, L=L, bank=bank: e.tensor_tensor(out=wmT[L][:].rearrange("p h t -> p (h t)"), in0=bank[:], in1=caus4[:], op=ALU.mult),
                  [tk, cz])
        PS.rel(k, [t_a])
        t_b = DVE(lambda e, L=L: e.tensor_scalar(out=gn[L][:], in0=gn_raw[:], scalar1=float(np.sqrt(128.0)), scalar2=None,
                                                  op0=ALU.mult), [lt])
        prev = [t_a, t_b]
        setup_toks += [t_a, t_b]
    setup_toks.append(c_tok)
    setup_toks += cz

    w_in_s = nc.dram_tensor("w_in_scr", [DEPTH, D, IN_COLS], BF16).ap()
    w_out_s = nc.dram_tensor("w_out_scr", [DEPTH, D, D], BF16).ap()
    fg_s = nc.dram_tensor("fg_scr", [D, D_FF], BF16).ap()
    fu_s = nc.dram_tensor("fu_scr", [D, D_FF], BF16).ap()
    fd_s = nc.dram_tensor("fd_scr", [D_FF, D], BF16).ap()
    s_cast = P.slot("s_cast")
    cast_jobs = []
    for L_ in range(DEPTH):
        cast_jobs.append((w_in_s[L_].rearrange("(a p) c -> p a c", p=128), w_in_d[L_].rearrange("(a p) c -> p a c", p=128)))
        cast_jobs.append((w_out_s[L_].rearrange("(a p) c -> p a c", p=128), w_out_d[L_].rearrange("(a p) c -> p a c", p=128)))
    cast_jobs.append((fg_s.rearrange("(a p) c -> p a c", p=128), fg_d[0].rearrange("(a p) c -> p a c", p=128)))
    cast_jobs.append((fu_s.rearrange("(a p) c -> p a c", p=128), fu_d[0].rearrange("(a p) c -> p a c", p=128)))
    cast_jobs.append((fd_s.rearrange("(a p) c -> p a c", p=128), fd_d[0].rearrange("(a p) c -> p a c", p=128)))

    hook = {"n": 0, "deferred": [], "it": 0}

    def issue_pre(n):
        if stop is not None or hook.get("pre") is None:
            return
        jobs, st_ = hook["pre"]
        while n > 0 and st_["left"] > 0 and jobs:
            o_, i_ = jobs.pop(0)
            P.dma("pool", o_, i_, hook["pre_slot"])
            st_["left"] -= 1
            n -= 1

    def wa_hook():
        hook["n"] += 1
        if hook["n"] % 2 == 0:
            issue_pre(1)
        if hook["n"] == 4 and hook["deferred"]:
            for f in hook["deferred"]:
                f()
            hook["deferred"] = []

    def load_WA(src_ap, ncols, _prefetch=False):
        if not _prefetch and hook.get("wa_pre"):
            r_ = hook["wa_pre"].pop(0)
            wa_hook()
            return r_
        ringA = cur_ring["WA"]
        k, tile, fr = ringA.get()
        if hook["it"] > 0:
            fr = list(fr) + [s_cast.tok()]
        if isinstance(src_ap, tuple) and src_ap[0] == "ind2":
            _, dram2d, idx_ap = src_ap
            P.raw("pool", lambda e: e.indirect_dma_start(out=tile[:].rearrange("p k c -> p (k c)"), out_offset=None, in_=dram2d,
                                                         in_offset=bass.IndirectOffsetOnAxis(ap=idx_ap, axis=0)), WA_s[k], waits=fr)
            if not _prefetch:
                wa_hook()
            return k, tile, WA_s[k].tok()
        if isinstance(src_ap, tuple):
            _, dram2d, idx_ap, col0 = src_ap
            for kc in range(KC):
                P.raw("pool", lambda e, kc=kc: e.indirect_dma_start(out=tile[:, kc, 0:ncols], out_offset=None, in_=dram2d,
                                                                   in_offset=bass.IndirectOffsetOnAxis(ap=idx_ap[:, kc:kc + 1], axis=0), element_offset=col0),
                      WA_s[k], waits=fr)
            wa_hook()
            return k, tile, WA_s[k].tok()
        if callable(src_ap):
            src = lambda e: src_ap(e).rearrange("(kc p) c -> p kc c", p=128)
        else:
            src = src_ap.rearrange("(kc p) c -> p kc c", p=128)
        P.dma("pool", tile[:, :, 0:ncols], src, WA_s[k], waits=fr)
        wa_hook()
        return k, tile, WA_s[k].tok()

    def load_WB(src_ap, nchunks):
        k, tile, fr = WB.get()
        if hook["it"] > 0:
            fr = list(fr) + [s_cast.tok()]
        if isinstance(src_ap, tuple) and src_ap[0] == "ind2":
            _, dram2d, idx_ap = src_ap
            P.raw("pool", lambda e: e.indirect_dma_start(out=tile[:].rearrange("p k c -> p (k c)"), out_offset=None, in_=dram2d,
                                                         in_offset=bass.IndirectOffsetOnAxis(ap=idx_ap, axis=0)), WB_s[k], waits=fr)
            return k, tile, WB_s[k].tok()
        if isinstance(src_ap, tuple):
            _, dram2d, idx_ap, _c = src_ap
            for jj in range(nchunks):
                P.raw("pool", lambda e, jj=jj: e.indirect_dma_start(out=tile[:, jj, :], out_offset=None, in_=dram2d,
                                                                   in_offset=bass.IndirectOffsetOnAxis(ap=idx_ap[:, jj:jj + 1], axis=0)),
                      WB_s[k], waits=fr)
            return k, tile, WB_s[k].tok()
        if callable(src_ap):
            src = lambda e: src_ap(e).rearrange("(c p) d -> p c d", p=128)
        else:
            src = src_ap.rearrange("(c p) d -> p c d", p=128)
        P.dma("pool", tile[:, 0:nchunks, :], src, WB_s[k], waits=fr)
        return k, tile, WB_s[k].tok()

    def load_lnp(g_ap, b_ap):
        k, tile, fr = lnp_ring.get()
        P.dma("sp", tile[:, 0, :], g_ap.partition_broadcast(128), lnp_slots[k], waits=fr)
        P.dma("sp", tile[:, 1, :], b_ap.partition_broadcast(128), lnp_slots[k], waits=fr)
        return k, tile, lnp_slots[k].tok()

    st = {"xres": [[] for _ in range(4)],
          "xT_tok": [[], []]}

    ln_st4 = sb("ln_st4", [128, 4, 2, 6], F32)
    ln_mv4 = sb("ln_mv4", [128, 4, 2], F32)
    ln_r4 = sb("ln_r4", [128, 4], F32)
    ln_l4 = sb("ln_l4", [128, 4], F32)
    ln_rd = [None] * 4

    def ln_A1(s, y_tok):
        ta = DVE(lambda e: e.bn_stats(out=ln_st4[:, s, 0, :], in_=x_res[:, s, 0:512]), [y_tok, ln_rd[s]])
        tb = DVE(lambda e: e.bn_stats(out=ln_st4[:, s, 1, :], in_=x_res[:, s, 512:1024]), [y_tok])
        tc_ = DVE(lambda e: e.bn_aggr(out=ln_mv4[:, s, :], in_=ln_st4[:, s, :, :].rearrange("p a b -> p (a b)")), [ta, tb])
        td0 = ACT(lambda e: e.activation(out=ln_l4[:, s:s + 1], in_=ln_mv4[:, s, 1:2], func=AF.Ln, bias=eps_ln[:, 0:1]), [tc_, eps_tok])
        td = ACT(lambda e: e.activation(out=ln_r4[:, s:s + 1], in_=ln_l4[:, s:s + 1], func=AF.Exp, scale=-0.5), [td0])
        return td

    def ln_partA(s, y_tok, lnp, lnp_tok):
        return ln_A2(s, ln_A1(s, y_tok), lnp, lnp_tok)

    def ln_A2(s, td, lnp, lnp_tok):
        xs = x_res[:, s, :]
        te = DVE(lambda e: e.tensor_scalar(out=xs, in0=xs, scalar1=ln_mv4[:, s, 0:1], scalar2=ln_r4[:, s:s + 1], op0=ALU.subtract, op1=ALU.mult), [td])
        ln_rd[s] = te
        h0, h1 = slice(0, 512), slice(512, 1024)
        tf0 = DVE(lambda e: e.tensor_tensor(out=x_res[:, s, h0], in0=x_res[:, s, h0], in1=lnp[:, 0, h0], op=ALU.mult), [te, lnp_tok])
        tg0 = DVE(lambda e: e.tensor_tensor(out=x_res[:, s, h0], in0=x_res[:, s, h0], in1=lnp[:, 1, h0], op=ALU.add), [tf0])
        tf1 = POOL(lambda e: e.tensor_tensor(out=x_res[:, s, h1], in0=x_res[:, s, h1], in1=lnp[:, 0, h1], op=ALU.mult), [te, lnp_tok])
        tg1 = POOL(lambda e: e.tensor_tensor(out=x_res[:, s, h1], in0=x_res[:, s, h1], in1=lnp[:, 1, h1], op=ALU.add), [tf1])
        return {"x": [tg0, tg1], "h": [tg0, tg1]}

    def ln_partB(s, A, xT_dst):
        evs = []
        for half in range(2):
            k, bank, fr = PS.get()
            tk = None
            for c in range(4):
                kc = half * 4 + c
                tk = PE(lambda e, kc=kc, c=c, bank=bank: e.transpose(bank[:, c * 128:(c + 1) * 128], x_res[:, s, kc * 128:(kc + 1) * 128], ident),
                        [A["h"][half], fr], sig=(c == 3))
            ev = ACT(lambda e, half=half, bank=bank: e.activation(out=xT_dst[:, half * 4:half * 4 + 4, s * 128:(s + 1) * 128],
                                                                   in_=bank[:].rearrange("p (c t) -> p c t", t=128), func=AF.Copy), [tk])
            PS.rel(k, [ev])
            evs.append(ev)
        return evs

    def ln_epilogue(s, y_tok, lnp, lnp_tok, xT_dst, final_out_rows=None, want_x2T=False, x2T_free=()):
        A = ln_partA(s, y_tok, lnp, lnp_tok)
        res = {"x": A["x"]}
        if final_out_rows is not None:
            return res
        res["xT"] = ln_partB(s, A, xT_dst)
        return res

    def mixer(L, it, xT_src, xT_src_toks, xT_dst, lnp_k, lnp, lnp_tok, no_xT=False):
        seq_start = (it * TT) % SEQ_T == 0
        cs = it * 0
        W = w_in_d[L] if (it == 0 or stop is not None) else w_in_s[L]
        WO = w_out_d[L] if (it == 0 or stop is not None) else w_out_s[L]
        s_tok = None
        if seq_start:
            s_tok = [DVE(lambda e: e.memset(S[L][:], 0.0), [mix_state[L]["S_rd"]]),
                     DVE(lambda e: e.memset(S_bf[L][:], 0.0), [mix_state[L]["Sbf_rd"]])]
            mix_state[L]["S_w"] = s_tok[0]
            mix_state[L]["Sbf_w"] = s_tok[1]
        k0, w0, w0t = load_WA(W[:, 0:512], 512)
        k1, w1, w1t = load_WA(W[:, 512:1024], 512)
        k2, w2, w2t = load_WA(W[:, 1024:1536], 512)
        u_toks = []
        for c in range(4):
            k, bank, fr = PS.get()
            tk = mm_group(bank[:], [(w0[:, kc, c * 128:(c + 1) * 128], xT_src[:, kc, :]) for kc in range(KC)], [w0t, xT_src_toks, fr])
            ev = ACT(lambda e, c=c, bank=bank: e.activation(out=uT[:, c, :], in_=bank[:], func=AF.Gelu), [tk])
            PS.rel(k, [ev])
            u_toks.append(ev)
        WA.rel(k0, [tk])
        vln_toks = []
        agg = []
        for s in range(4):
            k, bank, fr = PS.get()
            tk = mm_group(bank[:], [(xT_src[:, kc, s * 128:(s + 1) * 128], w1[:, kc, 0:512]) for kc in range(KC)], [w1t, fr])
            ev = ACT(lambda e, bank=bank, s=s: e.activation(out=vg4[:, s, :], in_=bank[:], func=AF.Gelu), [tk, mix_state[L].get("vg_rd")])
            PS.rel(k, [ev])
            ts = [DVE(lambda e, h=h, s=s: e.bn_stats(out=st6[:, s * 4 + h, :], in_=vg4[:, s, h * 128:(h + 1) * 128]), [ev]) for h in range(4)]
            agg += [DVE(lambda e, h=h, s=s: e.bn_aggr(out=mv[:, s * 4 + h, :], in_=st6[:, s * 4 + h, :]), [ts[h]]) for h in range(4)]
        WA.rel(k1, [tk])
        k3, w3, w3t = load_WA(W[:, 1536:2048], 512)
        qk_toks = []
        for which, dst in ((0, qT), (1, kT)):
            for h in range(4):
                k, bank, fr = PS.get()
                c0 = which * 256 + h * 64
                tk = mm_group(bank[0:64, :], [(w2[:, kc, c0:c0 + 64], xT_src[:, kc, :]) for kc in range(KC)], [w2t, fr, mix_state[L]["qk_rd"]])
                ev = ACT(lambda e, dst=dst, h=h, bank=bank: e.activation(out=dst[:, h, :], in_=bank[0:64, :], func=AF.Copy), [tk])
                PS.rel(k, [ev])
                qk_toks.append(ev)
        ktok_toks = []
        for s in range(4):
            k, bank, fr = PS.get()
            tk = mm_group(bank[:, 0:256], [(xT_src[:, kc, s * 128:(s + 1) * 128], w2[:, kc, 256:512]) for kc in range(KC)], [fr])
            ev = ACT(lambda e, s=s, bank=bank: e.activation(out=k_tok[:, s, :], in_=bank[:, 0:256], func=AF.Copy), [tk])
            PS.rel(k, [ev])
            ktok_toks.append(ev)
        WA.rel(k2, [tk])
        k4, w4, w4t = load_WA(W[:, 2048:2576], 528)
        vtok_toks = []
        for s in range(4):
            k, bank, fr = PS.get()
            tk = mm_group(bank[:], [(xT_src[:, kc, s * 128:(s + 1) * 128], w3[:, kc, 0:512]) for kc in range(KC)], [w3t, fr])
            ev = ACT(lambda e, s=s, bank=bank: e.activation(out=v_tok[:, s, :], in_=bank[:], func=AF.Copy), [tk])
            PS.rel(k, [ev])
            vtok_toks.append(ev)
        WA.rel(k3, [tk])
        t_lv = ACT(lambda e: e.activation(out=lnv16[:], in_=mv[:, :, 1], func=AF.Ln, bias=eps_ln[:, 0:1]), [agg, eps_tok])
        t_rs = ACT(lambda e: e.activation(out=rstd16[:], in_=lnv16[:], func=AF.Exp, scale=-0.5), [t_lv])
        last_vhat_rd = None
        for s in range(4):
            tn = [DVE(lambda e, h=h, s=s: e.tensor_scalar(out=vhat[:, h * 128:(h + 1) * 128], in0=vg4[:, s, h * 128:(h + 1) * 128],
                                                           scalar1=mv[:, s * 4 + h, 0:1], scalar2=rstd16[:, s * 4 + h:s * 4 + h + 1], op0=ALU.subtract, op1=ALU.mult),
                      [t_rs, last_vhat_rd]) for h in range(4)]
            tg_ = POOL(lambda e: e.tensor_tensor(out=vhat[:], in0=vhat[:], in1=lnG[L][:], op=ALU.mult), [tn, setup_toks])
            tb_ = POOL(lambda e, s=s: e.tensor_tensor(out=vln[:, s, :], in0=vhat[:], in1=lnBt[L][:], op=ALU.add), [tg_])
            last_vhat_rd = tb_
            vln_toks.append(tb_)
        mix_state[L]["vg_rd"] = tn[-1]
        k5, wo0, wo0t = load_WA(WO[:, 0:512], 512)
        k, bank, fr = PS.get()
        tk = mm_group(bank[0:16, :], [(w4[:, kc, 512:528], xT_src[:, kc, :]) for kc in range(KC)], [w4t, fr])
        a_tok = ACT(lambda e, bank=bank: e.activation(out=a_aug[0:16, :], in_=bank[0:16, :], func=AF.Copy), [tk, a_ones_tok])
        PS.rel(k, [a_tok])
        sg_toks = []
        for c in range(4):
            k, bank, fr = PS.get()
            tk = mm_group(bank[:], [(w4[:, kc, c * 128:(c + 1) * 128], xT_src[:, kc, :]) for kc in range(KC)], [fr])
            ev = ACT(lambda e, c=c, bank=bank: e.activation(out=sgT[:, c, :], in_=bank[:], func=AF.Silu), [tk])
            PS.rel(k, [ev])
            ev2 = DVE(lambda e, c=c: e.tensor_scalar(out=sgT[:, c, :], in0=sgT[:, c, :], scalar1=gn[L][:, c:c + 1], scalar2=None, op0=ALU.mult),
                      [ev, setup_toks])
            sg_toks.append(ev2)
        WA.rel(k4, [tk])
        k6, wo1, wo1t = load_WA(WO[:, 512:1024], 512)
        issue_pre(8)
        mix_state[L]["xT_rd"] = tk

        tm = mix_state[L]
        out_toks = []
        pendB = []
        pendA1 = []
        pendA2 = []

        def gate_head(s):
            sc = slice(s * 128, (s + 1) * 128)
            k, bank, fr = PS.get()
            tz = PE(lambda e, sc=sc, bank=bank: e.matmul(bank[:, 0:256], a_aug[0:17, sc], wa2[L][0:17, :], start=True, stop=True),
                    [a_tok, setup_toks, fr], sig=True)
            t_e1 = ACT(lambda e, bank=bank: e.activation(out=e1[:], in_=bank[:, 0:256], func=AF.Exp, scale=-1.0), [tz, tm.get("e1_rd")])
            PS.rel(k, [t_e1])
            t_L = ACT(lambda e: e.activation(out=Lsp[:], in_=e1[:], func=AF.Ln, bias=one_t[:, 0:1]), [t_e1, tm.get("L_rd"), eps_tok])
            tm["e1_rd"] = t_L
            tm["t_L"] = t_L

        for s in range(4):
            sc = slice(s * 128, (s + 1) * 128)
            if s == 0:
                gate_head(0)
            t_L = tm["t_L"]
            k, bcum, fr = PS.get()
            tcum = None
            for h in range(4):
                tcum = PE(lambda e, h=h, bcum=bcum: e.matmul(bcum[0:64, h * 128:(h + 1) * 128], Lsp[:, h * 64:(h + 1) * 64], tri, start=True, stop=True),
                          [t_L, fr], sig=(h == 3))
            k2_, brem, fr2 = PS.get()
            trem = PE(lambda e, brem=brem: e.matmul(brem[:, 0:256], triu, Lsp[:], start=True, stop=True), [fr2], sig=True)
            tm["L_rd"] = trem
            t_Eq = ACT(lambda e, bcum=bcum: e.activation(out=Eq[:].rearrange("p h t -> p (h t)"), in_=bcum[0:64, :], func=AF.Exp, scale=-1.0 / 16.0),
                       [tcum, tm.get("Eq_rd")])
            t_Ek = ACT(lambda e, bcum=bcum: e.activation(out=Ek[:].rearrange("p h t -> p (h t)"), in_=bcum[0:64, :], func=AF.Exp, scale=1.0 / 16.0),
                       [tcum, tm.get("Ek_rd")])
            PS.rel(k, [t_Eq, t_Ek])
            t_Er = ACT(lambda e, brem=brem: e.activation(out=Er[:], in_=brem[:, 0:256], func=AF.Exp, scale=-1.0 / 16.0), [trem, tm.get("Er_rd")])
            PS.rel(k2_, [t_Er])
            t_qd = DVE(lambda e, sc=sc: e.scalar_tensor_tensor(out=qd[:], in0=qT[:, :, sc], scalar=0.125, in1=Eq[:], op0=ALU.mult, op1=ALU.mult),
                       [t_Eq, qk_toks, tm.get("qd_rd")])
            t_ki = DVE(lambda e, sc=sc: e.tensor_tensor(out=kinv[:], in0=kT[:, :, sc], in1=Ek[:], op=ALU.mult), [t_Ek, qk_toks, tm.get("kinv_rd")])
            tm["Ek_rd"] = t_ki
            t_kd = DVE(lambda e, s=s: e.tensor_tensor(out=kdec[:], in0=k_tok[:, s, :], in1=Er[:], op=ALU.mult), [t_Er, ktok_toks[s], tm.get("kdec_rd")])
            tm["Er_rd"] = t_kd
            while pendA1:
                pendA1.pop(0)()
            k, bsc, fr = PS.get()
            tsc = None
            for h in range(4):
                tsc = PE(lambda e, h=h, bsc=bsc: e.matmul(bsc[:, h * 128:(h + 1) * 128], kinv[:, h, :], qd[:, h, :], start=True, stop=True),
                         [t_qd, t_ki, fr], sig=(h == 3))
            tm["kinv_rd"] = tsc
            t_scm = DVE(lambda e, bsc=bsc: e.tensor_tensor(out=scm[:].rearrange("p h t -> p (h t)"), in0=bsc[:], in1=tri4, op=ALU.mult),
                        [tsc, tm.get("scm_rd")])
            PS.rel(k, [t_scm])
            k, bank, fr = PS.get()
            tk = None
            for h in range(4):
                tk = PE(lambda e, h=h, s=s, bank=bank: e.matmul(bank[:, h * 128:(h + 1) * 128], vln[:, s, h * 128:(h + 1) * 128], wmT[L][:, h, :],
                                                                start=True, stop=True), [vln_toks[s], setup_toks, fr], sig=(h == 3))
            k_gm, bank_gm, tk_gm = k, bank, tk
            t_sa = DVE(lambda e, bank_gm=bank_gm: e.tensor_tensor(out=tmp_sa[:], in0=bank_gm[:], in1=bB[L][:], op=ALU.add), [tk_gm, tm.get("tmp_sa_rd")])
            PS.rel(k_gm, [t_sa])
            t_oa = DVE(lambda e, sc=sc: e.tensor_tensor(out=catT[:, 0:4, sc], in0=tmp_sa[:].rearrange("p (h t) -> p h t", t=128),
                                                         in1=uT[:, 0:4, sc], op=ALU.mult), [t_sa, u_toks])
            tm["tmp_sa_rd"] = t_oa
            k, bo, fr = PS.get()
            to = None
            for c in range(2):
                cc = slice(c * 64, (c + 1) * 64)
                for h in range(4):
                    PE(lambda e, h=h, c=c, bo=bo, cc=cc: e.matmul(bo[:, h * 128 + c * 64:h * 128 + (c + 1) * 64], S_bf[L][:, h, :], qd[:, h, cc],
                                                                   start=True, stop=False), [tm.get("Sbf_w"), t_scm, fr], sig=False)
                    to = PE(lambda e, h=h, c=c, bo=bo, cc=cc, s=s: e.matmul(bo[:, h * 128 + c * 64:h * 128 + (c + 1) * 64], v_tok[:, s, h * 128:(h + 1) * 128],
                                                                            scm[:, h, cc], start=False, stop=True), [vtok_toks[s]], sig=(h == 3))
                tm["Sbf_rd"] = to
                k3_, bd, fr3 = PS.get()
                td_ = None
                for h in range(4):
                    td_ = PE(lambda e, h=h, bd=bd, cc=cc, s=s: e.matmul(bd[0:64, h * 128:(h + 1) * 128], kdec[cc, h * 64:(h + 1) * 64],
                                                                        v_tok[cc, s, h * 128:(h + 1) * 128], start=True, stop=True), [t_kd, fr3], sig=(h == 3))
                tus = []
                for h in range(4):
                    tus.append(DVE(lambda e, h=h, bd=bd, c=c: e.scalar_tensor_tensor(out=S[L][:, h, :], in0=S[L][:, h, :], scalar=Eq[:, h, c * 64 + 63:c * 64 + 64],
                                                                                    in1=bd[0:64, h * 128:(h + 1) * 128], op0=ALU.mult, op1=ALU.add),
                                   [td_, t_Eq, tm.get("S_w")]))
                tm["S_w"] = tus[-1]
                PS.rel(k3_, tus)
                tcast = DVE(lambda e: e.tensor_copy(out=S_bf[L][:], in_=S[L][:]), [tus, tm.get("Sbf_rd")])
                tm["Sbf_w"] = tcast
                tm["S_rd"] = tcast
                if c == 0:
                    while pendA2:
                        pendA2.pop(0)()
            tm["qd_rd"] = to
            tm["kdec_rd"] = td_
            tm["scm_rd"] = to
            tm["Eq_rd"] = tus[-1]
            t_sq = ACT(lambda e, bo=bo: e.activation(out=sq_bf[:], in_=bo[:], func=AF.Square), [to, tm.get("sq_rd")])
            k4_, bss, fr4 = PS.get()
            tss = PE(lambda e, bss=bss: e.matmul(bss[:], ones_bf[:], sq_bf[:], start=True, stop=True), [t_sq, fr4, ones_bf_tok], sig=True)
            tm["sq_rd"] = tss
            t_r0 = ACT(lambda e, bss=bss: e.activation(out=rr_l[:], in_=bss[:], func=AF.Ln, bias=eps_rms[:, 0:1]), [tss, eps_tok])
            t_r = ACT(lambda e: e.activation(out=rr[:], in_=rr_l[:], func=AF.Exp, scale=-0.5), [t_r0, tm.get("rr_rd")])
            PS.rel(k4_, [t_r0])
            while pendB:
                pendB.pop(0)()
            if s + 1 < 4:
                gate_head(s + 1)
            t_t1 = DVE(lambda e, bo=bo: e.tensor_tensor(out=t1[:], in0=bo[:], in1=rr[:], op=ALU.mult), [t_r, tm.get("t1_rd")])
            PS.rel(k, [t_t1, t_sq])
            tm["rr_rd"] = t_t1
            t_ob = DVE(lambda e, sc=sc: e.tensor_tensor(out=catT[:, 4:8, sc], in0=t1[:].rearrange("p (h t) -> p h t", t=128), in1=sgT[:, 0:4, sc], op=ALU.mult),
                       [t_t1, sg_toks])
            tm["t1_rd"] = t_ob
            ys = []
            for half, (wo, wot) in enumerate(((wo0, wo0t), (wo1, wo1t))):
                k, bank, fr = PS.get()
                tk = mm_group(bank[:], [(catT[:, c, sc], wo[:, c, 0:512]) for c in range(8)], [t_oa, t_ob, wot, fr])
                ty = DVE(lambda e, half=half, bank=bank, s=s: e.scalar_tensor_tensor(out=x_res[:, s, half * 512:(half + 1) * 512],
                                                                                    in0=x_res[:, s, half * 512:(half + 1) * 512], scalar=ALPHA,
                                                                                    in1=bank[:], op0=ALU.mult, op1=ALU.add), [tk, st["xres"][s]])
                PS.rel(k, [ty])
                ys.append(ty)
            last_wo = tk
            r = {"x": None, "xT": []}
            out_toks.append(r)

            def A1(s=s, ys=ys, r=r):
                r["td"] = ln_A1(s, ys)

            def A2(s=s, r=r):
                A_ = ln_A2(s, r["td"], lnp, lnp_tok)
                r["x"] = A_["x"]
                st["xres"][s] = list(A_["x"])
                if not no_xT:
                    def flushB(s=s, A_=A_, r=r):
                        r["xT"] = ln_partB(s, A_, xT_dst)
                        st["xres"][s] = list(A_["x"]) + r["xT"]
                    pendB.append(flushB)
            pendA1.append(A1)
            pendA2.append(A2)
        while pendA1:
            pendA1.pop(0)()
        while pendA2:
            pendA2.pop(0)()
        while pendB:
            pendB.pop(0)()
        WA.rel(k5, [last_wo])
        WA.rel(k6, [last_wo])
        tm["qk_rd"] = tm["qd_rd"]
        return out_toks

    def ffn(xT_src, xT_toks, wsrc, F, evac_fn, mid_hook=None, moe_q=None):
        nfc = F // 128
        hook["n"] = 0
        groups = []
        c0 = 0
        while c0 < F:
            gw = min(512, F - c0)
            groups.append((c0, gw))
            c0 += gw
        h_toks = []
        tk = None
        wb_plan = []
        c_ = 0
        while c_ < nfc:
            n_ = min(4, nfc - c_)
            wb_plan.append((c_, n_))
            c_ += n_
        wb_loaded = {}

        def issue_wb(i):
            if moe_q is not None:
                return
            if i < len(wb_plan) and i not in wb_loaded:
                cc_, nn_ = wb_plan[i]
                wb_loaded[i] = load_WB(wsrc('d', cc_ * 128, (cc_ + nn_) * 128), nn_)
        for gi_, (c0, gw) in enumerate(groups):
            if moe_q is not None:
                kg, wg, wgt = moe_q.pop('g')
                ku, wu, wut = moe_q.pop('u')
                wg = wg.rearrange("p (k c) -> p k c", c=512)
                wu = wu.rearrange("p (k c) -> p k c", c=512)
            else:
                kg, wg, wgt = load_WA(wsrc('g', c0, c0 + gw), gw)
                ku, wu, wut = load_WA(wsrc('u', c0, c0 + gw), gw)
            for j in range(gw // 128):
                fc = c0 // 128 + j
                k1_, bg, fr1 = PS.get()
                tg = mm_group(bg[:], [(wg[:, kc, j * 128:(j + 1) * 128], xT_src[:, kc, :]) for kc in range(KC)], [wgt, xT_toks, fr1])
                k2_, bu, fr2 = PS.get()
                tu = mm_group(bu[:], [(wu[:, kc, j * 128:(j + 1) * 128], xT_src[:, kc, :]) for kc in range(KC)], [wut, fr2])
                ks, sgt, frs = sg_ring.get()
                ta = ACT(lambda e, bg=bg, sgt=sgt: e.activation(out=sgt[:], in_=bg[:], func=AF.Silu), [tg, frs])
                PS.rel(k1_, [ta])
                th = DVE(lambda e, bu=bu, sgt=sgt, fc=fc: e.tensor_tensor(out=hT[:, fc, :], in0=bu[:], in1=sgt[:], op=ALU.mult), [tu, ta])
                PS.rel(k2_, [th])
                sg_ring.rel(ks, [th])
                h_toks.append(th)
            if moe_q is not None:
                moe_q.rel(kg, [tu])
                moe_q.rel(ku, [tu])
                if gi_ == 1:
                    for f in hook["deferred"]:
                        f()
                    hook["deferred"] = []
                continue
            cur_ring["WA"].rel(kg, [tu])
            cur_ring["WA"].rel(ku, [tu])
            if gi_ == min(2, len(groups) - 1):
                issue_wb(0)
                issue_wb(1)
        banks = [PS.get() for _ in range(8)]
        last = None
        for wi_, (c, n) in enumerate(wb_plan):
            issue_wb(wi_)
            if wi_ == 3 and mid_hook is not None:
                mid_hook()
            if moe_q is not None:
                kd, wd, wdt = moe_q.pop('d')
                wd = wd.rearrange("p (k c) -> p k c", c=1024)
            else:
                kd, wd, wdt = wb_loaded[wi_]
            for j in range(n):
                fc = c + j
                for s in range(4):
                    for half in range(2):
                        kb, bank, fr = banks[s * 2 + half]
                        last = PE(lambda e, bank=bank, fc=fc, s=s, half=half, wd=wd, j=j: e.matmul(bank[:], hT[:, fc, s * 128:(s + 1) * 128],
                                                                                                 wd[:, j, half * 512:(half + 1) * 512],
                                                                                                 start=(fc == 0), stop=(fc == nfc - 1)),
                                  [wdt, h_toks[fc], fr], sig=(fc == nfc - 1) or (j == n - 1 and s == 3 and half == 1))
                        if fc == nfc - 1:
                            banks[s * 2 + half] = (kb, bank, last)
            if moe_q is not None:
                moe_q.rel(kd, [last])
            else:
                WB.rel(kd, [last])
        res = []
        for s in range(4):
            for half in range(2):
                kb, bank, tok = banks[s * 2 + half]
                tv = evac_fn(s, half, bank, tok)
                PS.rel(kb, [tv])
                res.append(tv)
        return res

    mix_state = [dict(S_rd=None, Sbf_rd=None, qk_rd=None) for _ in range(DEPTH)]
    a_ones_tok = POOL(lambda e: e.memset(a_aug[:], 1.0), [])
    eps_tok = [POOL(lambda e: e.memset(eps_ln[:], LN_EPS), []), POOL(lambda e: e.memset(eps_rms[:], 128.0 * RMS_EPS), []), POOL(lambda e: e.memset(one_t[:], 1.0), [])]
    wdesc_tok = DVE(lambda e: e.tensor_scalar(out=wdesc[:], in0=consts[:, C_JROW:C_JROW + NE], scalar1=-1.0, scalar2=float(NE), op0=ALU.mult, op1=ALU.add), [c_tok])
    s_x = P.slot("s_x")
    s_o = P.slot("s_out")
    out_slots = s_o

    def dump(it):
        for s in range(4):
            P.dma("sp", out_d[it * TT + s * 128: it * TT + (s + 1) * 128, :], x_res[:, s, :], s_o, waits=[st["xres"][s]])

    NSUB = NT // 128
    NSLOT = (2 * NT + 8 * (TT - 1)) // TT
    dk = dict(kind="ExternalOutput") if debug else {}
    x2_d = nc.dram_tensor("x2_scr", [NT + 128, D], F32, **dk).ap()
    y_d = nc.dram_tensor("y_scr", [2 * NT + NSLOT * TT, D], F32, **dk).ap()
    sinfo_d = nc.dram_tensor("sinfo_scr", [NSLOT * TT, 4], I32, **dk).ap()
    sel_all = sb("sel_all", [128, NSUB, NE], F32)
    eq1_all = sb("eq1_all", [128, NSUB, NE], F32)
    gates_all = sb("gates_all", [128, NSUB, NE], F32)
    s_x2 = [P.slot(f"s_x2_{i}") for i in range(4)]
    s_x2z = P.slot("s_x2z")
    router_toks = []
    NG7 = D_FFE // 512
    wg_s = nc.dram_tensor("wg_scr", [NE * NG7 * 128, 4096], BF16).ap()
    wu_s = nc.dram_tensor("wu_scr", [NE * NG7 * 128, 4096], BF16).ap()
    wd_s = nc.dram_tensor("wd_scr", [NE * NG7 * 128, 4096], BF16).ap()
    s_pre = P.slot("s_pre")
    pre_jobs = []
    for e_ in range(NE):
        for g_ in range(NG7):
            r0 = (e_ * NG7 + g_) * 128
            pre_jobs.append((wg_s[r0:r0 + 128, :].rearrange("p (k c) -> p k c", c=512), eg_d[0, e_][:, g_ * 512:(g_ + 1) * 512].rearrange("(kc p) c -> p kc c", p=128)))
            pre_jobs.append((wu_s[r0:r0 + 128, :].rearrange("p (k c) -> p k c", c=512), eu_d[0, e_][:, g_ * 512:(g_ + 1) * 512].rearrange("(kc p) c -> p kc c", p=128)))
            pre_jobs.append((wd_s[r0:r0 + 128, :].rearrange("p (k c) -> p k c", c=1024), ed_d[0, e_][g_ * 512:(g_ + 1) * 512, :].rearrange("(c p) d -> p c d", p=128)))
    pre_per_tile = (len(pre_jobs) + max(ntiles - 1, 1) - 1) // max(ntiles - 1, 1)

    def dense_w(kind, a, b):
        src = (fg_d[0], fu_d[0], fd_d[0]) if hook["it"] == 0 else (fg_s, fu_s, fd_s)
        if kind == "g":
            return src[0][:, a:b]
        if kind == "u":
            return src[1][:, a:b]
        return src[2][a:b, :]

    for it in range(ntiles):
        for s in range(4):
            P.dma("sp", x_res[:, s, :], x_d[it * TT + s * 128: it * TT + (s + 1) * 128, :], s_x, waits=[st["xres"][s]])
        xtok = s_x.tok()
        xT_toks = []
        for s in range(4):
            for half in range(2):
                k, bank, fr = PS.get()
                tk = None
                for c in range(4):
                    kc = half * 4 + c
                    tk = PE(lambda e, kc=kc, c=c, bank=bank, s=s: e.transpose(bank[:, c * 128:(c + 1) * 128], x_res[:, s, kc * 128:(kc + 1) * 128], ident),
                            [xtok, c_tok, fr], sig=(c == 3))
                ev = ACT(lambda e, half=half, bank=bank, s=s: e.activation(out=xT[0][:, half * 4:half * 4 + 4, s * 128:(s + 1) * 128],
                                                                            in_=bank[:].rearrange("p (c t) -> p c t", t=128), func=AF.Copy), [tk])
                PS.rel(k, [ev])
                xT_toks.append(ev)
            st["xres"][s] = [xtok, tk]
        cur = 0
        stopped = False
        hook["it"] = it if stop is None else 0
        if it == 0 and stop is None:
            hook["pre"] = (cast_jobs, {"left": len(cast_jobs)})
            hook["pre_slot"] = s_cast
        else:
            hook["pre"] = (pre_jobs, {"left": pre_per_tile})
            hook["pre_slot"] = s_pre
        for L in range(DEPTH):
            is_moe = (L % 2 == 1)
            lk, lnp, lnp_tok = load_lnp(ln_mix_g_d[L], ln_mix_b_d[L])
            r = mixer(L, it, xT[cur], xT_toks, xT[1 - cur], lk, lnp, lnp_tok, no_xT=(is_moe and stop is None))
            lnp_ring.rel(lk, [r[-1]["x"]])
            cur = 1 - cur
            xT_toks = [t for rr_ in r for t in rr_["xT"]]
            if stop == ("mix", L):
                stopped = True
                break
            if not is_moe:
                lk, lnp, lnp_tok = load_lnp(ln_ffn_g_d[L], ln_ffn_b_d[L])

                def evac(s, half, bank, tok):
                    return DVE(lambda e: e.scalar_tensor_tensor(out=x_res[:, s, half * 512:(half + 1) * 512], in0=x_res[:, s, half * 512:(half + 1) * 512],
                                                                 scalar=ALPHA, in1=bank[:], op0=ALU.mult, op1=ALU.add), [tok, st["xres"][s]])
                ys = ffn(xT[cur], xT_toks, dense_w, D_FF, evac)
                rs = []
                tds = [ln_A1(s, [ys[s * 2], ys[s * 2 + 1]]) for s in range(4)]
                As = [ln_A2(s, tds[s], lnp, lnp_tok) for s in range(4)]
                for s in range(4):
                    r = {"x": As[s]["x"], "xT": ln_partB(s, As[s], xT[1 - cur])}
                    st["xres"][s] = list(r["x"]) + r["xT"]
                    rs.append(r)
                lnp_ring.rel(lk, [rs[-1]["x"]])
                cur = 1 - cur
                xT_toks = [t for rr_ in rs for t in rr_["xT"]]
                if stop == ("ffn", L):
                    stopped = True
                    break
            else:
                x2T_free = None
                prev_t10 = None
                for s in range(4):
                    g = it * 4 + s
                    evs = []
                    for half in range(2):
                        k, bank, fr = PS.get()
                        tk = None
                        for c in range(4):
                            kc = half * 4 + c
                            tk = PE(lambda e, kc=kc, c=c, bank=bank, s=s: e.transpose(bank[:, c * 128:(c + 1) * 128], x_res[:, s, kc * 128:(kc + 1) * 128], ident),
                                    [st["xres"][s], fr], sig=(c == 3))
                        ev = ACT(lambda e, half=half, bank=bank: e.activation(out=x2T[:, half * 4:half * 4 + 4, :],
                                                                               in_=bank[:].rearrange("p (c t) -> p c t", t=128), func=AF.Copy), [tk, x2T_free])
                        PS.rel(k, [ev])
                        evs.append(ev)
                    k, bank, fr = PS.get()
                    tl = mm_group(bank[:, 0:NE], [(x2T[:, kc, :], rw[:, kc, :]) for kc in range(KC)], [evs, c_tok, fr])
                    x2T_free = tl
                    t0 = DVE(lambda e, bank=bank: e.tensor_copy(out=lg[:], in_=bank[:, 0:NE]), [tl, prev_t10])
                    PS.rel(k, [t0])
                    t1_ = DVE(lambda e: e.reduce_max(out=m1[:], in_=lg[:], axis=AX.X), [t0])
                    t2a = DVE(lambda e: e.tensor_scalar(out=eq1[:], in0=lg[:], scalar1=m1[:, 0:1], scalar2=None, op0=ALU.is_ge), [t1_])
                    t2b = DVE(lambda e: e.tensor_tensor(out=eq1[:], in0=eq1[:], in1=wdesc[:], op=ALU.mult), [t2a, wdesc_tok])
                    t2c = DVE(lambda e: e.reduce_max(out=mt[:], in_=eq1[:], axis=AX.X), [t2b])
                    t2_ = DVE(lambda e, g=g: e.tensor_scalar(out=eq1_all[:, g, :], in0=eq1[:], scalar1=mt[:, 0:1], scalar2=None, op0=ALU.is_ge), [t2c])
                    t3_ = DVE(lambda e, g=g: e.scalar_tensor_tensor(out=l2[:], in0=eq1_all[:, g, :], scalar=-1e30, in1=lg[:], op0=ALU.mult, op1=ALU.add), [t2_])
                    t4_ = DVE(lambda e: e.reduce_max(out=m2[:], in_=l2[:], axis=AX.X), [t3_])
                    t5a = DVE(lambda e: e.tensor_scalar(out=sel[:], in0=l2[:], scalar1=m2[:, 0:1], scalar2=None, op0=ALU.is_ge), [t4_])
                    t5b = DVE(lambda e: e.tensor_tensor(out=sel[:], in0=sel[:], in1=wdesc[:], op=ALU.mult), [t5a])
                    t5c = DVE(lambda e: e.reduce_max(out=mt[:], in_=sel[:], axis=AX.X), [t5b])
                    t5d = DVE(lambda e: e.tensor_scalar(out=sel[:], in0=sel[:], scalar1=mt[:, 0:1], scalar2=None, op0=ALU.is_ge), [t5c])
                    t5_ = DVE(lambda e, g=g: e.tensor_tensor(out=sel_all[:, g, :], in0=sel[:], in1=eq1_all[:, g, :], op=ALU.add), [t5d])
                    t6_ = DVE(lambda e: e.tensor_scalar(out=nm1[:], in0=m1[:], scalar1=-1.0, scalar2=None, op0=ALU.mult), [t1_])
                    t7_ = ACT(lambda e: e.activation(out=ex[:], in_=lg[:], func=AF.Exp, bias=nm1[:, 0:1], scale=1.0), [t6_, t0])
                    t8_ = ACT(lambda e: e.activation(out=e2[:], in_=m2[:], func=AF.Exp, bias=nm1[:, 0:1], scale=1.0), [t6_, t4_])
                    t9a = DVE(lambda e: e.tensor_scalar(out=e2[:], in0=e2[:], scalar1=1.0, scalar2=None, op0=ALU.add), [t8_])
                    t9_ = DVE(lambda e: e.reciprocal(out=rden[:], in_=e2[:]), [t9a])
                    t10 = DVE(lambda e, g=g: e.scalar_tensor_tensor(out=gates_all[:, g, :], in0=ex[:], scalar=rden[:, 0:1], in1=sel_all[:, g, :], op0=ALU.mult, op1=ALU.mult),
                              [t9_, t7_, t5_])
                    prev_t10 = t10
                    router_toks.append(t10)
                    P.dma("sp", x2_d[g * 128:(g + 1) * 128, :], x_res[:, s, :], s_x2[s], waits=[st["xres"][s]])
                    st["xres"][s] = [s_x2[s].tok(), tk]
        if stopped:
            for s in range(4):
                P.dma("sp", out_d[it * TT + s * 128: it * TT + (s + 1) * 128, :], x_res[:, s, :], s_o, waits=[st["xres"][s]])
            out_tok_now = s_o.tok()
            for s in range(4):
                st["xres"][s] = [out_tok_now]

    hook["pre"] = None
    hook["it"] = 0
    if stop is None:
        while cast_jobs:
            o_, i_ = cast_jobs.pop(0)
            P.dma("pool", o_, i_, s_cast)
        while pre_jobs:
            o_, i_ = pre_jobs.pop(0)
            P.dma("pool", o_, i_, s_pre)
        W8 = NSUB * NE
        for e_ in ("dve", "pool", "act", "pe"):
            pt = [(P.sems[q], P.cnt[q]) for q in ("pe", "act", "dve", "pool")]
            P.op(e_, lambda e: e.nop(), pt, sig=False)
        mf_off = [512]

        def mf(shape, dt=F32):
            n = int(np.prod(shape))
            ap = mixF[:, mf_off[0]:mf_off[0] + n]
            mf_off[0] += n
            assert mf_off[0] <= 6400
            if dt is not F32:
                ap = ap.bitcast(dt)
            if len(shape) == 2:
                return ap.rearrange("p (a b) -> p a b", b=shape[1])
            if len(shape) == 3:
                return ap.rearrange("p (a b c) -> p a b c", b=shape[1], c=shape[2])
            return ap
        R1s = mf([NSUB, NE]); Tots = mf([NSUB, NE]); cs = mf([NSUB, NE]); pos = mf([NSUB, NE]); prod = mf([NSUB, NE]); oh2 = mf([NSUB, NE])
        cnt = mf([NE]); nsl = mf([NE]); tmp8 = mf([NE]); base = mf([NE]); endv = mf([NE]); posb = mf([NE])
        posk_f = mf([2, NSUB]); gatek = mf([2, NSUB]); posk_i = mf([2, NSUB], I32)
        info = mf([2, NSUB, 4], I32)
        eidf = mf([32]); tmpj = mf([32]); eid_i = mf([32], I32)
        padt = mf([NSLOT * 4, 4], I32)
        sinfo = [mf([4, 4], I32) for i in range(3)]
        sinfo_s = [P.slot(f"sinfo_s{i}") for i in range(3)]
        e1024 = mf([32])
        widx_g = mf([NSLOT, 8], I32)
        flat = lambda t: t[:].rearrange("p a b -> p (a b)")

        k, bank, fr = PS.get()
        tR = PE(lambda e, bank=bank: e.matmul(bank[:, 0:W8], slt, flat(sel_all), start=True, stop=True), [router_toks, c_tok, fr], sig=True)
        k2, bank2, fr2 = PS.get()
        tT = PE(lambda e, bank2=bank2: e.matmul(bank2[:, 0:W8], ones, flat(sel_all), start=True, stop=True), [fr2], sig=True)
        tR1 = DVE(lambda e, bank=bank: e.tensor_copy(out=flat(R1s), in_=bank[:, 0:W8]), [tR])
        PS.rel(k, [tR1])
        tTo = DVE(lambda e, bank2=bank2: e.tensor_copy(out=flat(Tots), in_=bank2[:, 0:W8]), [tT])
        PS.rel(k2, [tTo])
        tp_ = DVE(lambda e: e.memset(cs[:, 0, :], 0.0), [])
        for sub in range(1, NSUB):
            tp_ = DVE(lambda e, sub=sub: e.tensor_tensor(out=cs[:, sub, :], in0=cs[:, sub - 1, :], in1=Tots[:, sub - 1, :], op=ALU.add), [tp_, tTo])
        t_cnt = DVE(lambda e: e.tensor_tensor(out=cnt[:], in0=cs[:, NSUB - 1, :], in1=Tots[:, NSUB - 1, :], op=ALU.add), [tp_, tTo])
        tn_ = DVE(lambda e: e.memset(nsl[:], 0.0), [])
        for kk in range(8):
            ta_ = DVE(lambda e, kk=kk: e.tensor_scalar(out=tmp8[:], in0=cnt[:], scalar1=float(TT * kk) + 0.5, scalar2=None, op0=ALU.is_ge), [t_cnt, tn_])
            tn_ = DVE(lambda e: e.tensor_tensor(out=nsl[:], in0=nsl[:], in1=tmp8[:], op=ALU.add), [ta_])
        tb_ = DVE(lambda e: e.memset(base[:, 0:1], 0.0), [])
        for ee in range(1, NE):
            tb_ = DVE(lambda e, ee=ee: e.tensor_tensor(out=base[:, ee:ee + 1], in0=base[:, ee - 1:ee], in1=nsl[:, ee - 1:ee], op=ALU.add), [tb_, tn_])
        t_end = DVE(lambda e: e.tensor_tensor(out=endv[:], in0=base[:], in1=nsl[:], op=ALU.add), [tb_, tn_])
        t_pb = DVE(lambda e: e.tensor_scalar(out=posb[:], in0=base[:], scalar1=float(TT), scalar2=None, op0=ALU.mult), [tb_])
        t_tp = DVE(lambda e: e.tensor_tensor(out=flat(prod), in0=flat(cs), in1=flat(R1s), op=ALU.add), [tp_, tR1])
        t_pos = None
        tl_ = []
        for ee in range(NE):
            tl_.append(DVE(lambda e, ee=ee: e.tensor_scalar(out=pos[:, :, ee], in0=prod[:, :, ee], scalar1=posb[:, ee:ee + 1], scalar2=None, op0=ALU.add),
                           [t_tp, t_pb]))
        t_oh2 = DVE(lambda e: e.tensor_tensor(out=flat(oh2), in0=flat(sel_all), in1=flat(eq1_all), op=ALU.subtract), [router_toks])
        t_info0 = DVE(lambda e: e.memset(info[:].rearrange("p a b c -> p (a b c)"), 0), [])
        tprev = tl_[-1]
        info_toks = []
        for kk, oh in ((0, eq1_all), (1, oh2)):
            t_a = DVE(lambda e, oh=oh: e.tensor_tensor(out=flat(prod), in0=flat(oh), in1=flat(pos), op=ALU.mult), [tl_, t_oh2, tprev])
            t_b = DVE(lambda e, kk=kk: e.reduce_sum(out=posk_f[:, kk, :], in_=prod[:], axis=AX.X), [t_a])
            t_c = DVE(lambda e, oh=oh: e.tensor_tensor(out=flat(prod), in0=flat(oh), in1=flat(gates_all), op=ALU.mult), [t_b])
            t_d = DVE(lambda e, kk=kk: e.reduce_sum(out=gatek[:, kk, :], in_=prod[:], axis=AX.X), [t_c])
            tprev = t_d
            t_e = DVE(lambda e, kk=kk: e.tensor_copy(out=posk_i[:, kk, :], in_=posk_f[:, kk, :]), [t_b])
            t_f = DVE(lambda e, kk=kk: e.tensor_copy(out=info[:, kk, :, 0], in_=consts[:, C_TOK:C_TOK + NSUB]), [t_info0, c_tok])
            t_g = DVE(lambda e, kk=kk: e.tensor_scalar(out=info[:, kk, :, 1], in0=consts[:, C_TOK:C_TOK + NSUB], scalar1=float(kk * NT), scalar2=None, op0=ALU.add),
                      [t_info0, c_tok])
            t_h = DVE(lambda e, kk=kk: e.tensor_copy(out=info[:, kk, :, 2].bitcast(F32), in_=gatek[:, kk, :]), [t_d, t_info0])
            info_toks += [t_e, t_f, t_g, t_h]
        te_ = DVE(lambda e: e.memset(eidf[:], 0.0), [])
        for ee in range(NE):
            tj_ = DVE(lambda e, ee=ee: e.tensor_scalar(out=tmpj[:], in0=jrow, scalar1=endv[:, ee:ee + 1], scalar2=None, op0=ALU.is_ge), [t_end, te_, c_tok])
            te_ = DVE(lambda e: e.tensor_tensor(out=eidf[:], in0=eidf[:], in1=tmpj[:], op=ALU.add), [tj_])
        te_ = DVE(lambda e: e.tensor_scalar(out=eidf[:], in0=eidf[:], scalar1=float(NE - 1), scalar2=None, op0=ALU.min), [te_])
        t_eid = DVE(lambda e: e.tensor_copy(out=eid_i[:], in_=eidf[:]), [te_])
        t_e1 = DVE(lambda e: e.tensor_scalar(out=e1024[:], in0=eidf[:], scalar1=float(NG7 * 128), scalar2=None, op0=ALU.mult), [te_])
        widx_toks = []
        for j in range(NSLOT):
            widx_toks.append(DVE(lambda e, j=j: e.tensor_scalar(out=widx_g[:, j, 0:NG7], in0=consts[:, C_IG:C_IG + NG7], scalar1=e1024[:, j:j + 1], scalar2=None, op0=ALU.add),
                                 [t_e1, c_tok]))
        t_pad0 = DVE(lambda e: e.memset(padt[:].rearrange("p a b -> p (a b)"), 0), [])
        t_pad1 = DVE(lambda e: e.tensor_scalar(out=padt[:, :, 0], in0=consts[:, C_PADQ:C_PADQ + NSLOT * 4], scalar1=0.0, scalar2=float(NT), op0=ALU.mult, op1=ALU.add),
                     [t_pad0, c_tok])
        t_pad = DVE(lambda e: e.tensor_copy(out=padt[:, :, 1], in_=consts[:, C_PADQ:C_PADQ + NSLOT * 4]), [t_pad0, c_tok])
        s_pad = P.slot("s_pad")
        pad_tok = P.dma("sp", sinfo_d.rearrange("(p r) w -> p r w", p=128), padt[:], s_pad, waits=[t_pad, t_pad1])
        t_zr = DVE(lambda e: e.memset(vhat[:], 0.0), [mix_state[DEPTH - 1].get("vg_rd")])
        P.dma("sp", x2_d[NT:NT + 128, 0:512], vhat[:], s_x2z, waits=[t_zr])
        P.dma("sp", x2_d[NT:NT + 128, 512:1024], vhat[:], s_x2z, waits=[t_zr])
        s_sc = P.slot("s_scat")
        for kk in range(2):
            for sub in range(NSUB):
                P.raw("pool", lambda e, kk=kk, sub=sub: e.indirect_dma_start(out=sinfo_d, out_offset=bass.IndirectOffsetOnAxis(ap=posk_i[:, kk, sub:sub + 1], axis=0),
                                                                          in_=info[:, kk, sub, :], in_offset=None),
                      s_sc, waits=[pad_tok, info_toks])
        scat_tok = s_sc.tok()
        x2_all_fn = lambda: [q.tok() for q in s_x2] + [s_x2z.tok()]
        x2_all = None
        holder = {}
        s_g = P.slot("s_gather")
        s_y = P.slot("s_yscat")
        xg_free = [None]
        si_ring = Ring(sinfo)

        def prep_slot(j):
            kq, sit, frq = si_ring.get()
            tsi = P.dma("sp", sit[:], sinfo_d[j * TT:(j + 1) * TT, :].rearrange("(r p) w -> p r w", p=128), sinfo_s[kq], waits=[scat_tok, frq])
            tz = [xg_free[0]]
            if j == 0:
                tz = POOL(lambda e: e.memset(qk_buf[:, :], 0.0), [xg_free[0], mix_state[DEPTH - 1].get("qk_rd"), mix_state[DEPTH - 1].get("kinv_rd")])
            for r_ in range(4):
                P.raw("pool", lambda e, r_=r_, sit=sit: e.indirect_dma_start(out=xg4[:, r_, :], out_offset=None, in_=x2_d,
                                                                            in_offset=bass.IndirectOffsetOnAxis(ap=sit[:, r_, 0:1], axis=0)),
                      s_g, waits=[tsi, tz, x2_all_fn()])
            return kq, sit, tsi, s_g.tok()

        def transposes_slot(j, gtok):
            dst = xT[j % 2]
            toks = []
            tk = None
            for r_ in range(4):
                for half in range(2):
                    k, bank, fr = PS.get()
                    for c in range(4):
                        kc = half * 4 + c
                        tk = PE(lambda e, kc=kc, c=c, bank=bank, r_=r_: e.transpose(bank[:, c * 128:(c + 1) * 128], xg4[:, r_, kc * 128:(kc + 1) * 128], ident),
                                [gtok, fr], sig=(c == 3))
                    ev = ACT(lambda e, half=half, bank=bank, r_=r_, dst=dst: e.activation(out=dst[:, half * 4:half * 4 + 4, r_ * 128:(r_ + 1) * 128],
                                                                                         in_=bank[:].rearrange("p (c t) -> p c t", t=128), func=AF.Copy), [tk])
                    PS.rel(k, [ev])
                    toks.append(ev)
            xg_free[0] = tk
            return toks

        prep = {0: prep_slot(0)}
        cur_xT = transposes_slot(0, prep[0][3])
        hook["wa_pre"] = []
        hook["deferred"] = []

        class WQ:
            def __init__(self):
                self.tiles = [WAall[:, i * 4096:(i + 1) * 4096] for i in range(4)] + [WB.tiles[i][:].rearrange("p k c -> p (k c)") for i in range(2)]
                self.slots = list(WA_s) + list(WB_s)
                self.free = [list(widx_toks) + [s_pre.tok()] for _ in range(4)] + [list(f) + list(widx_toks) + [s_pre.tok()] for f in WB.free]
                self.seq = [(j, kind, gi) for j in range(NSLOT)
                            for (kind, gi) in ([(kk_, g_) for g_ in range(NG7) for kk_ in ("g", "u")] + [("d", g_) for g_ in range(NG7)])]
                self.pos = 0
                self.fifo = []
                for _ in range(6):
                    self.issue()

            def issue(self):
                if self.pos >= len(self.seq):
                    return
                j, kind, gi = self.seq[self.pos]
                k = self.pos % 6
                self.pos += 1
                src = {"g": wg_s, "u": wu_s, "d": wd_s}[kind]
                tile = self.tiles[k]
                idx = widx_g[:, j, gi:gi + 1]
                P.raw("pool", lambda e: e.indirect_dma_start(out=tile, out_offset=None, in_=src,
                                                             in_offset=bass.IndirectOffsetOnAxis(ap=idx, axis=0)), self.slots[k], waits=self.free[k])
                self.fifo.append((k, tile, self.slots[k].tok(), kind))

            def pop(self, kind):
                k, tile, tok, kd = self.fifo.pop(0)
                assert kd == kind, (kd, kind)
                return k, tile, tok

            def rel(self, k, toks):
                self.free[k] = [t for t in toks if t is not None]
                self.issue()

        wq = WQ()

        def moe_w_for(j):
            def moe_w(kind, a, b):
                gi_ = a // 512
                src = {"g": wg_s, "u": wu_s, "d": wd_s}[kind]
                return ("ind2", src, widx_g[:, j, gi_:gi_ + 1])
            return moe_w

        for j in range(NSLOT):
            kq, sit, tsi, _ = prep[j]
            if j + 1 < NSLOT:
                hook["deferred"].append(lambda j=j: prep.__setitem__(j + 1, prep_slot(j + 1)))
            moe_w = moe_w_for(j)

            def mid(j=j):
                if j + 1 < NSLOT:
                    w_ = moe_w_for(j + 1)
                    for (kind_, a_) in (("g", 0), ("u", 0), ("g", 512), ("u", 512)):
                        hook["wa_pre"].append(load_WA(w_(kind_, a_, a_ + 512), 512, _prefetch=True))

            def evac(s, half, bank, tok, sit=sit, tsi=tsi):
                return DVE(lambda e: e.tensor_scalar(out=x_res[:, s, half * 512:(half + 1) * 512], in0=bank[:], scalar1=sit[:, s, 2:3].bitcast(F32), scalar2=None,
                                                     op0=ALU.mult), [tok, tsi, st["xres"][s]])
            ys = ffn(xT[j % 2], cur_xT, moe_w, D_FFE, evac, moe_q=wq)
            if j + 1 < NSLOT:
                cur_xT = transposes_slot(j + 1, prep[j + 1][3])

            def do_scatter(ys=ys, sit=sit, kq=kq):
                for r_ in range(4):
                    P.raw("pool", lambda e, r_=r_: e.indirect_dma_start(out=y_d, out_offset=bass.IndirectOffsetOnAxis(ap=sit[:, r_, 1:2], axis=0),
                                                                      in_=x_res[:, r_, :], in_offset=None),
                          s_y, waits=[ys[r_ * 2], ys[r_ * 2 + 1]])
                tk_ = s_y.tok()
                for r_ in range(4):
                    st["xres"][r_] = [tk_]
                si_ring.rel(kq, [tk_])
            if j + 1 < NSLOT:
                hook["deferred"].append(do_scatter)
            else:
                do_scatter()
        y_all = s_y.tok()
        if debug:
            dbg_d = nc.dram_tensor("dbg", [128, 32 + NSLOT * 36 + 32 + 8 * 4], I32, kind="ExternalOutput").ap()
            s_dbg = P.slot("s_dbg")
            P.dma("sp", dbg_d[:, 0:32], eid_i[:], s_dbg, waits=[t_eid])
            P.dma("sp", dbg_d[:, 32:32 + NSLOT * 8], widx_g[:].rearrange("p a b -> p (a b)"), s_dbg, waits=[widx_toks])
            P.dma("sp", dbg_d[:, 32 + NSLOT * 36:32 + NSLOT * 36 + 32], eidf[:].bitcast(I32), s_dbg, waits=[t_eid])
            for q_, tt_ in enumerate((cnt, nsl, base, endv)):
                P.dma("sp", dbg_d[:, 64 + NSLOT * 36 + q_ * 8:64 + NSLOT * 36 + (q_ + 1) * 8], tt_[:].bitcast(I32), s_dbg, waits=[t_eid])
            dbg2_d = nc.dram_tensor("dbg2", [128, 4, 1024], F32, kind="ExternalOutput").ap()
            dbg3_d = nc.dram_tensor("dbg3", [128, 4, 1024], F32, kind="ExternalOutput").ap()
            dbg4_d = nc.dram_tensor("dbg4", [128, 8, 512], F32, kind="ExternalOutput").ap()
            P.dma("sp", dbg2_d, xg4[:], s_dbg, waits=[s_g.tok()])
            P.dma("sp", dbg3_d, x_res[:], s_dbg, waits=[st["xres"]])
            tq = None
            for kc_ in range(8):
                tq = ACT(lambda e, kc_=kc_: e.activation(out=vg4[:, 0, :], in_=xT[0][:, kc_, :], func=AF.Copy), [cur_xT, tq, s_dbg.tok()])
                P.dma("sp", dbg4_d[:, kc_, :], vg4[:, 0, :], s_dbg, waits=[tq])
                tq = s_dbg.tok()
            y_all = [y_all, s_dbg.tok()]
        lk, lnp, lnp_tok = load_lnp(ln_ffn_g_d[DEPTH - 1], ln_ffn_b_d[DEPTH - 1])
        ovlF = ovl[:, :].bitcast(F32)
        yb_ring = Ring([xg4[:, 0:2, :], xg4[:, 2:4, :], ovlF[:, 0:2048].rearrange("p (a b) -> p a b", b=1024),
                        ovlF[:, 2048:4096].rearrange("p (a b) -> p a b", b=1024)])
        s_f = [P.slot(f"s_fin{i}") for i in range(4)]
        s_yl = [P.slot(f"s_yl{i}") for i in range(4)]
        s_of = [P.slot(f"s_of{i}") for i in range(4)]
        last_x = None
        PF = 2

        def issue_loads(g):
            s = g % 4
            tx = P.dma("sp", x_res[:, s, :], x2_d[g * 128:(g + 1) * 128, :], s_f[s], waits=[st["xres"][s], y_all, x2_all_fn()])
            ky, yt_, fry = yb_ring.get()
            P.dma("sp", yt_[:, 0, :], y_d[g * 128:(g + 1) * 128, :], s_yl[ky], waits=[fry, y_all, xg_free[0]])
            ty = P.dma("sp", yt_[:, 1, :], y_d[NT + g * 128:NT + (g + 1) * 128, :], s_yl[ky], waits=[fry, y_all])
            return tx, ty, ky, yt_
        loads = {}
        for g in range(min(PF, NSUB)):
            loads[g] = issue_loads(g)
        for g in range(NSUB):
            s = g % 4
            if g + PF < NSUB:
                loads[g + PF] = issue_loads(g + PF)
            tx, ty, ky, yt_ = loads.pop(g)
            ta_ = DVE(lambda e, s=s, yt_=yt_: e.scalar_tensor_tensor(out=x_res[:, s, :], in0=x_res[:, s, :], scalar=ALPHA, in1=yt_[:, 0, :], op0=ALU.mult, op1=ALU.add), [tx, ty])
            tb2 = DVE(lambda e, s=s, yt_=yt_: e.tensor_tensor(out=x_res[:, s, :], in0=x_res[:, s, :], in1=yt_[:, 1, :], op=ALU.add), [ta_])
            yb_ring.rel(ky, [tb2])
            r = ln_epilogue(s, [tb2], lnp, lnp_tok, None, final_out_rows=True)
            last_x = r["x"]
            fo = P.dma("act", out_d[g * 128:(g + 1) * 128, :], x_res[:, s, :], s_of[s], waits=[r["x"]])
            st["xres"][s] = [fo]
        lnp_ring.rel(lk, [last_x])
        final_toks = [q.tok() for q in s_of]
    P.emit([s_o.tok()] + (final_toks if stop is None else []))
    return nc


_PARAM_NAMES = ["w_in", "gm_ws", "gm_bs", "gm_ln_g", "gm_ln_b", "gla_wa2", "gla_ba", "gla_norm_g", "w_out", "ln_mix_g", "ln_mix_b",
                "ffn_w_gate", "ffn_w_up", "ffn_w_down", "router_w", "exp_w_gate", "exp_w_up", "exp_w_down", "ln_ffn_g", "ln_ffn_b"]


def run(inputs, NT, SEQ_T, ncores, stop=None, debug=False):
    nc = build(NT, SEQ_T, stop, debug)
    x = np.ascontiguousarray(np.asarray(inputs["x"], dtype=np.float32)).reshape(-1, D)
    base = {}
    for n in _PARAM_NAMES:
        a = np.ascontiguousarray(np.asarray(inputs[n], dtype=np.float32))
        if n == "gm_bs":
            a = a.reshape(DEPTH, 512)
        base[n] = a
    base["consts"] = make_consts(NT)
    in_maps = []
    for c in range(ncores):
        m = dict(base)
        m["x"] = np.ascontiguousarray(x[c * NT:(c + 1) * NT])
        in_maps.append(m)
    res = run_bass_kernel_spmd(nc, in_maps, core_ids=list(range(ncores)))
    return np.concatenate([r["out"] for r in res.results], axis=0), res


def kernel(**inputs):
    x = np.asarray(inputs["x"])
    B, S_, _ = x.shape
    NT = (B * S_) // NCORES
    out, _ = run(inputs, NT, S_, NCORES)
    return out.reshape(B, S_, D).astype(np.float32)
```
